# Optimizing a Trainium2 kernel written in Bass

```python
import math
import jax
import jax.numpy as jnp
from jax import lax
import numpy as np

D_MODEL = 2048
BATCH = 8
SEQ = 2048
DEPTH = 1

HEAD_DIM = 128
FOX_HEADS = 8
RET_HEADS = 8
FOX_WIDTH = FOX_HEADS * HEAD_DIM
RET_WIDTH = RET_HEADS * HEAD_DIM
N_BRANCH = 2
Q_BLOCK = 128
RET_CHUNK = 128
N_GROUPS = 4
EXPERTS_PER_GROUP = 8
N_EXPERTS = N_GROUPS * EXPERTS_PER_GROUP
TOP_K = 2
D_EXPERT = 512
EXPERT_BLOCK = 128
ROPE_BASE = 10000.0
EPS = 1e-6
IN_WIDTH = 3 * FOX_WIDTH + FOX_HEADS + 4 * RET_WIDTH + N_BRANCH * D_MODEL

kernel_name = 'fox_retnet_hier_moe_block'


def rmsnorm(x, w):
    xf = x.astype(jnp.float32)
    y = xf * lax.rsqrt(jnp.mean(xf * xf, axis=-1, keepdims=True) + EPS)
    return (y * w.astype(jnp.float32)).astype(x.dtype)


def to_heads(t, n_heads):
    b, s, _ = t.shape
    return t.reshape(b, s, n_heads, HEAD_DIM).transpose(0, 2, 1, 3)


def from_heads(t):
    b, h, s, d = t.shape
    return t.transpose(0, 2, 1, 3).reshape(b, s, h * d)


def rotary(t, positions):
    half = HEAD_DIM // 2
    inv_freq = ROPE_BASE ** (-jnp.arange(half, dtype=jnp.float32) / half)
    ang = positions.astype(jnp.float32)[:, None, :, None] * inv_freq
    cos, sin = jnp.cos(ang), jnp.sin(ang)
    tf = t.astype(jnp.float32)
    t1, t2 = tf[..., :half], tf[..., half:]
    return jnp.concatenate([t1 * cos - t2 * sin, t1 * sin + t2 * cos], axis=-1).astype(t.dtype)


def forgetting_attention(q, k, v, log_f):
    b, h, s, d = q.shape
    nq = s // Q_BLOCK
    scale = 1.0 / math.sqrt(d)
    c = jnp.cumsum(log_f, axis=-1)
    qb = q.reshape(b, h, nq, Q_BLOCK, d).transpose(2, 0, 1, 3, 4)
    cb = c.reshape(b, h, nq, Q_BLOCK).transpose(2, 0, 1, 3)
    key_pos = jnp.arange(s)

    def block(args):
        qi, ci, i = args
        logits = jnp.einsum('bhqd,bhkd->bhqk', qi, k).astype(jnp.float32) * scale
        logits = logits + ci[..., None] - c[:, :, None, :]
        q_pos = i * Q_BLOCK + jnp.arange(Q_BLOCK)
        mask = key_pos[None, :] <= q_pos[:, None]
        logits = jnp.where(mask[None, None], logits, jnp.float32(-1e30))
        p = jax.nn.softmax(logits, axis=-1)
        return jnp.einsum('bhqk,bhkd->bhqd', p.astype(v.dtype), v)

    out = lax.map(block, (qb, cb, jnp.arange(nq)))
    return out.transpose(1, 2, 0, 3, 4).reshape(b, h, s, d)


def retention_chunkwise(q, k, v):
    b, h, s, d = q.shape
    nc = s // RET_CHUNK
    log_gamma = jnp.log1p(-(2.0 ** (-5.0 - jnp.arange(h, dtype=jnp.float32))))
    n = jnp.arange(RET_CHUNK, dtype=jnp.float32)
    diff = n[:, None] - n[None, :]
    decay_in = jnp.where(diff[None] >= 0, jnp.exp(diff[None] * log_gamma[:, None, None]), 0.0)
    xi = jnp.exp((n[None, :] + 1.0) * log_gamma[:, None])
    zeta = jnp.exp((RET_CHUNK - 1.0 - n[None, :]) * log_gamma[:, None])
    g_chunk = jnp.exp(RET_CHUNK * log_gamma)
    qf = q.astype(jnp.float32) / math.sqrt(d)
    kf = k.astype(jnp.float32)
    vf = v.astype(jnp.float32)
    chunks = lambda t: t.reshape(b, h, nc, RET_CHUNK, d).transpose(2, 0, 1, 3, 4)

    def step(state, inp):
        qc, kc, vc = inp
        inner = jnp.einsum('bhnd,bhmd->bhnm', qc, kc) * decay_in[None]
        o = jnp.einsum('bhnm,bhme->bhne', inner, vc)
        o = o + jnp.einsum('bhnd,bhde->bhne', qc, state) * xi[None, :, :, None]
        state = state * g_chunk[None, :, None, None] + jnp.einsum(
            'bhmd,bhme->bhde', kc * zeta[None, :, :, None], vc)
        return state, o

    state0 = jnp.zeros((b, h, d, d), jnp.float32)
    _, out = lax.scan(step, state0, (chunks(qf), chunks(kf), chunks(vf)))
    return out.transpose(1, 2, 0, 3, 4).reshape(b, h, s, d)


def hierarchical_moe(h, w_rg, b_rg, w_re, b_re, w1, w3, w2):
    t_tok, d = h.shape
    g_logits = (h @ w_rg + b_rg).astype(jnp.float32)
    g_prob = jax.nn.softmax(g_logits, axis=-1)
    g_sel = jnp.argmax(g_prob, axis=-1)
    p_group = jnp.take_along_axis(g_prob, g_sel[:, None], axis=-1)[:, 0]
    e_logits = (h @ w_re + b_re).astype(jnp.float32).reshape(t_tok, N_GROUPS, EXPERTS_PER_GROUP)
    e_logits = jnp.take_along_axis(e_logits, g_sel[:, None, None], axis=1)[:, 0]
    e_prob = jax.nn.softmax(e_logits, axis=-1)
    top_p, top_i = lax.top_k(e_prob, TOP_K)
    top_p = top_p / jnp.sum(top_p, axis=-1, keepdims=True)
    weights = p_group[:, None] * top_p
    expert_id = g_sel[:, None] * EXPERTS_PER_GROUP + top_i

    m = t_tok * TOP_K
    e_flat = expert_id.reshape(m).astype(jnp.int32)
    w_flat = weights.reshape(m)
    tok_flat = jnp.repeat(jnp.arange(t_tok, dtype=jnp.int32), TOP_K)
    order = jnp.argsort(e_flat)
    e_s, tok_s, w_s = e_flat[order], tok_flat[order], w_flat[order]
    counts = jnp.zeros((N_EXPERTS,), jnp.int32).at[e_flat].add(1)
    padded = (counts + EXPERT_BLOCK - 1) // EXPERT_BLOCK * EXPERT_BLOCK
    starts = jnp.cumsum(counts) - counts
    pends = jnp.cumsum(padded)
    pstarts = pends - padded
    dest = pstarts[e_s] + (jnp.arange(m, dtype=jnp.int32) - starts[e_s])
    cap = m + N_EXPERTS * EXPERT_BLOCK
    n_blk = cap // EXPERT_BLOCK
    buf_tok = jnp.full((cap,), t_tok, jnp.int32).at[dest].set(tok_s)
    buf_w = jnp.zeros((cap,), h.dtype).at[dest].set(w_s.astype(h.dtype))
    blk_expert = jnp.clip(jnp.searchsorted(pends, jnp.arange(n_blk, dtype=jnp.int32) * EXPERT_BLOCK,
                                           side='right'), 0, N_EXPERTS - 1)
    h_pad = jnp.concatenate([h, jnp.zeros((1, d), h.dtype)], axis=0)
    xb = h_pad[buf_tok].reshape(n_blk, EXPERT_BLOCK, d)

    def expert_block(args):
        xi, e = args
        return (jax.nn.silu(xi @ w1[e]) * (xi @ w3[e])) @ w2[e]

    yb = lax.map(expert_block, (xb, blk_expert)).reshape(cap, d) * buf_w[:, None]
    return jnp.zeros((t_tok + 1, d), h.dtype).at[buf_tok].add(yb)[:t_tok]


def setup_inputs(seed: int = 0) -> dict:
    key = jax.random.key(seed)
    ks = jax.random.split(key, 20)
    f32 = jnp.float32
    nrm = lambda k, shape, scale: jax.random.normal(k, shape, f32) * scale
    x = jax.random.normal(ks[0], (BATCH, SEQ, D_MODEL), f32)
    offsets = jax.random.randint(ks[1], (BATCH, 1), 0, 1024, dtype=jnp.int32)
    positions = offsets + jnp.arange(SEQ, dtype=jnp.int32)[None, :]
    return {
        'x': x,
        'positions': positions,
        'norm_mix_w': 1.0 + nrm(ks[2], (DEPTH, D_MODEL), 0.02),
        'w_in': nrm(ks[3], (DEPTH, D_MODEL, IN_WIDTH), D_MODEL ** -0.5),
        'fox_b_f': 2.0 + nrm(ks[4], (DEPTH, FOX_HEADS), 0.1),
        'ret_gn_w': 1.0 + nrm(ks[5], (DEPTH, RET_WIDTH), 0.02),
        'w_branch': nrm(ks[6], (DEPTH, N_BRANCH, FOX_WIDTH, D_MODEL), FOX_WIDTH ** -0.5),
        'b_gate': nrm(ks[7], (DEPTH, N_BRANCH, D_MODEL), 0.01),
        'w_out': nrm(ks[8], (DEPTH, D_MODEL, D_MODEL), D_MODEL ** -0.5),
        'norm_ffn_w': 1.0 + nrm(ks[9], (DEPTH, D_MODEL), 0.02),
        'w_router_group': nrm(ks[10], (DEPTH, D_MODEL, N_GROUPS), D_MODEL ** -0.5),
        'b_router_group': nrm(ks[11], (DEPTH, N_GROUPS), 0.01),
        'w_router_expert': nrm(ks[12], (DEPTH, D_MODEL, N_EXPERTS), D_MODEL ** -0.5),
        'b_router_expert': nrm(ks[13], (DEPTH, N_EXPERTS), 0.01),
        'w1': nrm(ks[14], (DEPTH, N_EXPERTS, D_MODEL, D_EXPERT), D_MODEL ** -0.5),
        'w3': nrm(ks[15], (DEPTH, N_EXPERTS, D_MODEL, D_EXPERT), D_MODEL ** -0.5),
        'w2': nrm(ks[16], (DEPTH, N_EXPERTS, D_EXPERT, D_MODEL), D_EXPERT ** -0.5),
        'norm_final_w': 1.0 + nrm(ks[17], (D_MODEL,), 0.02),
    }


def reference(x, positions, norm_mix_w, w_in, fox_b_f, ret_gn_w, w_branch, b_gate, w_out,
              norm_ffn_w, w_router_group, b_router_group, w_router_expert, b_router_expert,
              w1, w3, w2, norm_final_w):
    b, s, d = x.shape
    split_at = [FOX_WIDTH, 2 * FOX_WIDTH, 3 * FOX_WIDTH, 3 * FOX_WIDTH + FOX_HEADS,
                3 * FOX_WIDTH + FOX_HEADS + RET_WIDTH, 3 * FOX_WIDTH + FOX_HEADS + 2 * RET_WIDTH,
                3 * FOX_WIDTH + FOX_HEADS + 3 * RET_WIDTH, 3 * FOX_WIDTH + FOX_HEADS + 4 * RET_WIDTH]
    h = x
    for l in range(DEPTH):
        xn = rmsnorm(h, norm_mix_w[l])
        proj = xn @ w_in[l]
        fq, fk, fv, f_logit, rq, rk, rv, rg, gate_logit = jnp.split(proj, split_at, axis=-1)
        log_f = jax.nn.log_sigmoid((f_logit + fox_b_f[l]).astype(jnp.float32)).transpose(0, 2, 1)
        fox_o = from_heads(forgetting_attention(to_heads(fq, FOX_HEADS), to_heads(fk, FOX_HEADS),
                                                to_heads(fv, FOX_HEADS), log_f))
        ret = retention_chunkwise(rotary(to_heads(rq, RET_HEADS), positions),
                                  rotary(to_heads(rk, RET_HEADS), positions),
                                  to_heads(rv, RET_HEADS))
        mu = jnp.mean(ret, axis=-1, keepdims=True)
        var = jnp.mean(jnp.square(ret - mu), axis=-1, keepdims=True)
        ret = from_heads((ret - mu) * lax.rsqrt(var + EPS)) * ret_gn_w[l].astype(jnp.float32)
        ret_o = (jax.nn.silu(rg.astype(jnp.float32)) * ret).astype(h.dtype)
        branches = jnp.stack([fox_o, ret_o], axis=2)
        branch_d = jnp.einsum('bsnc,ncd->bsnd', branches, w_branch[l])
        gates = jax.nn.sigmoid(gate_logit.reshape(b, s, N_BRANCH, d) + b_gate[l])
        merged = jnp.sum(gates * branch_d, axis=2)
        h = h + merged @ w_out[l]
        hn = rmsnorm(h, norm_ffn_w[l]).reshape(b * s, d)
        moe = hierarchical_moe(hn, w_router_group[l], b_router_group[l], w_router_expert[l],
                               b_router_expert[l], w1[l], w3[l], w2[l])
        h = h + moe.reshape(b, s, d)
    return rmsnorm(h, norm_final_w)
```

```python
import contextlib
import math
import os
import numpy as np
import concourse.bass as bass
import concourse.mybir as mybir
from concourse.bass_utils import run_bass_kernel_spmd

F32 = mybir.dt.float32
BF16 = mybir.dt.bfloat16
I32 = mybir.dt.int32
AF = mybir.ActivationFunctionType
ALU = mybir.AluOpType
AX = mybir.AxisListType

T = 2048
D = 2048
NT = 16
KC = 16
HD = 128
NH = 8
IN_W = 11272
C_FQ, C_FK, C_FV, C_FL = 0, 1024, 2048, 3072
C_RQ, C_RK, C_RV, C_RG, C_GATE = 3080, 4104, 5128, 6152, 7176
NE = 32
DE = 512
CAP = 256
NSLOT = NE * CAP
EPS = 1e-6
PI = math.pi


class Reg:
    __slots__ = ("w", "r")

    def __init__(self):
        self.w = None
        self.r = {}


class KB:
    NDS = 56

    def __init__(self, nc, es):
        self.nc = nc
        self.E = {"pe": nc.tensor, "act": nc.scalar, "dve": nc.vector, "pool": nc.gpsimd, "sp": nc.sync}
        self.sem = {k: es.enter_context(nc.semaphore("sem_" + k)) for k in ("pe", "act", "dve")}
        self.cnt = {k: 0 for k in self.sem}
        self.dsem = [es.enter_context(nc.semaphore(f"dsem{i}")) for i in range(self.NDS)]
        self.dcnt = [0] * self.NDS
        self.drr = {"sp": 0, "pool": 0}
        self.tick = None
        self.pace = 28
        self.seen = {k: {} for k in self.E}
        self.nins = 0
        self.nwait = 0

    def _semof(self, key):
        return self.sem[key] if isinstance(key, str) else self.dsem[key]

    def _wait(self, eng, deps):
        best = {}
        for k, n in deps:
            if best.get(k, 0) < n:
                best[k] = n
        sn = self.seen[eng]
        for k, n in best.items():
            if sn.get(k, 0) >= n:
                continue
            self.E[eng].wait_ge(self._semof(k), n)
            self.nwait += 1
            sn[k] = n

    def _deps(self, eng, rd, wr):
        deps = []
        for r in rd:
            if r.w is not None and not (eng == "pe" and r.w[0] == "pe"):
                deps.append(r.w)
        for w in wr:
            if w.w is not None and w.w[0] != eng:
                deps.append(w.w)
            for k, n in w.r.items():
                if k != eng:
                    deps.append((k, n))
        return deps

    def _commit(self, tok, rd, wr):
        k, n = tok
        for r in rd:
            if r.r.get(k, 0) < n:
                r.r[k] = n
        for w in wr:
            w.w = tok
            w.r = {}

    def op(self, eng, fn, rd=(), wr=()):
        self._wait(eng, self._deps(eng, rd, wr))
        ins = fn()
        self.cnt[eng] += 1
        self.nins += 1
        ins.then_inc(self.sem[eng], 1)
        tok = (eng, self.cnt[eng])
        self._commit(tok, rd, wr)
        if eng == "pe" and self.tick is not None and self.cnt["pe"] % self.pace == 0:
            self.tick()
        return tok

    def dma(self, q, fn, rd=(), wr=()):
        half = self.NDS // 2
        i = (self.drr[q] % half) + (0 if q == "sp" else half)
        self.drr[q] += 1
        deps = self._deps(q, rd, wr)
        if self.dcnt[i] > 0:
            deps.append((i, self.dcnt[i]))
        self._wait(q, deps)
        ins = fn(self.E[q])
        self.dcnt[i] += 16
        self.nins += 1
        ins.then_inc(self.dsem[i], 16)
        tok = (i, self.dcnt[i])
        self._commit(tok, rd, wr)
        return tok

    def barrier(self, engs=("pe", "act", "dve", "pool", "sp")):
        deps = [(k, self.cnt[k]) for k in self.sem if self.cnt[k] > 0]
        deps += [(i, self.dcnt[i]) for i in range(self.NDS) if self.dcnt[i] > 0]
        for e in engs:
            self._wait(e, deps)


def _interleave(ga, gb, ratio):
    a_alive = ga is not None
    b_alive = gb is not None
    while a_alive or b_alive:
        if a_alive:
            for _ in range(ratio):
                try:
                    next(ga)
                except StopIteration:
                    a_alive = False
                    break
        if b_alive:
            try:
                next(gb)
            except StopIteration:
                b_alive = False


DBG = int(os.environ.get("KDBG", "0"))
NCONV = int(os.environ.get("KNCONV", "32"))


def build(stop="all", taps=()):
    nc = bass.Bass("TRN2", target_bir_lowering=False)

    def din(name, shape, dt=F32):
        return nc.dram_tensor(name, list(shape), dt, kind="ExternalInput").ap()

    def dscr(name, shape, dt):
        return nc.dram_tensor(name, list(shape), dt, kind="Internal").ap()

    x_d = din("x", [T, D])
    pos_d = din("pos", [1, T], I32)
    w_in_d = din("w_in", [D, IN_W])
    w_br_d = din("w_branch", [2, 1024, D])
    w_out_d = din("w_out", [D, D])
    wr_d = din("w_router", [D, 36])
    w1_d = din("w1", [NE, D, DE])
    w3_d = din("w3", [NE, D, DE])
    w2_d = din("w2", [NE, DE, D])
    nmw_d = din("nmwT", [128, KC])
    bf_d = din("bf_bc", [128, 128])
    gnw_d = din("gnwT", [128, NH])
    bg_d = din("bgT", [128, 32])
    nfw_d = din("nfw_bc", [128, D])
    nfin_d = din("nfin_bc", [128, D])
    rb_d = din("rbias_bc", [128, 36])
    cmat_d = din("cmat", [128, 6 * 128])
    cret_d = din("cret", [128, 2 * NH * 128 + NH + 2])
    ebase_d = din("ebase", [128, NT * NE])
    out_d = nc.dram_tensor("out", [T, D], F32, kind="ExternalOutput").ap()

    foxo_d = dscr("foxo_d", [NH, 128, T], BF16)
    reto_d = dscr("reto_d", [NH, 128, T], BF16)
    gates_d = dscr("gates_d", [2 * D, T], BF16)
    h_d = dscr("h_d", [T, D], F32)
    hn_d = dscr("hn_d", [T, D], BF16)
    xg_d = dscr("xg_d", [NSLOT + 128, D], BF16)
    y_d = dscr("y_d", [NSLOT + 128, D], BF16)

    wb1_d = dscr("wb1_d", [NE, 128, KC * DE], BF16)
    wb3_d = dscr("wb3_d", [NE, 128, KC * DE], BF16)
    wb2_d = dscr("wb2_d", [NE, 128, 4 * D], BF16)

    tap_out = {}

    es = contextlib.ExitStack()
    with es:
        kb = KB(nc, es)
        regs = {}

        def R(name):
            r = regs.get(name)
            if r is None:
                r = regs[name] = Reg()
            return r

        uniq = {"i": 0}

        def sbt(stack, name, shape, dt=F32):
            uniq["i"] += 1
            return stack.enter_context(nc.sbuf_tensor(f"s{uniq['i']}_{name}", list(shape), dt))

        ps = [es.enter_context(nc.psum_tensor(f"ps{i}", [128, 512], F32)) for i in range(8)]
        psb = [p[:].bitcast(BF16) for p in ps]
        Rps = [R(f"ps{i}") for i in range(8)]

        cmat_f = sbt(es, "cmat_f", [128, 6 * 128], F32)
        cmat_b = sbt(es, "cmat_b", [128, 6 * 128], BF16)
        kb.dma("sp", lambda q: q.dma_start(out=cmat_f[:], in_=cmat_d), wr=[R("cmat_f")])
        kb.dma("pool", lambda q: q.dma_start(out=cmat_b[:], in_=cmat_d), wr=[R("cmat_b")])
        identb = cmat_b[:, 0:128]
        maskTb = cmat_b[:, 128:256]
        Lstrb = cmat_b[:, 256:384]
        onesb = cmat_b[:, 384:512]
        Rswapb = cmat_b[:, 512:640]
        Ucumf = cmat_f[:, 128:256]
        onesf = cmat_f[:, 384:512]
        sel127f = cmat_f[:, 640:768]
        RCB = R("cmat_b")
        RCF = R("cmat_f")
        eps_t = sbt(es, "eps_t", [128, 1], F32)
        one_t = sbt(es, "one_t", [128, 1], F32)
        kb.op("dve", lambda: nc.vector.memset(eps_t[:], EPS), wr=[R("eps_t")])
        kb.op("dve", lambda: nc.vector.memset(one_t[:], 1.0), wr=[R("one_t")])

        NSTG = 6
        LAG = 2
        stg = [sbt(es, f"stg{i}", [128, 1024], BF16) for i in range(NSTG)]
        conv_units = []
        for e in range(NCONV):
            for q8 in range(8):
                conv_units.append((e, "w1", q8))
                conv_units.append((e, "w3", q8))
                conv_units.append((e, "w2", q8))
        cstate = {"k": 0}

        def conv_in(k):
            e, mat, q8 = conv_units[k]
            sbuf = stg[k % NSTG]
            if mat == "w2":
                m, hf = q8 // 2, q8 % 2
                src = w2_d[e][m * 128:(m + 1) * 128, hf * 1024:(hf + 1) * 1024]
                dst = sbuf[:, :]
            else:
                wd = w1_d if mat == "w1" else w3_d
                src = wd[e][q8 * 256:(q8 + 1) * 256, :].rearrange("(kc p) n -> p kc n", p=128)
                dst = sbuf[:, :].rearrange("p (kc n) -> p kc n", n=DE)
            kb.dma("pool", lambda q: q.dma_start(out=dst, in_=src), wr=[R(f"stg{k % NSTG}")])

        def conv_out(k):
            e, mat, q8 = conv_units[k]
            sbuf = stg[k % NSTG]
            wd = {"w1": wb1_d, "w3": wb3_d, "w2": wb2_d}[mat]
            kb.dma("sp", lambda q: q.dma_start(out=wd[e][:, q8 * 1024:(q8 + 1) * 1024], in_=sbuf[:, :]),
                   rd=[R(f"stg{k % NSTG}")], wr=[R(f"wbu_{e}_{mat}_{q8}")])

        def conv_tick():
            k = cstate["k"]
            if k >= len(conv_units) + LAG:
                return
            if k >= LAG:
                conv_out(k - LAG)
            if k < len(conv_units):
                conv_in(k)
            cstate["k"] = k + 1

        def conv_flush():
            while cstate["k"] < len(conv_units) + LAG:
                conv_tick()

        NWR = 3
        pA = contextlib.ExitStack()
        es.enter_context(pA)
        wring = [sbt(pA, f"wring{i}", [128, KC, 256], BF16) for i in range(NWR)]
        Rwr = [R(f"wring{i}") for i in range(NWR)]
        wstate = {"n": 0}

        def load_w(c0, ncols=256):
            s = wstate["n"] % NWR
            wstate["n"] += 1
            src = w_in_d[:, c0:c0 + ncols].rearrange("(kc p) n -> p kc n", p=128)
            kb.dma("pool", lambda q: q.dma_start(out=wring[s][:, :, 0:ncols], in_=src), wr=[Rwr[s]])
            return s

        bank_rr = {"i": 0}

        def tap(name, sb_ap, shape, dt, rd):
            if name not in taps:
                return
            t = nc.dram_tensor("tap_" + name, list(shape), dt, kind="ExternalOutput").ap()
            tap_out[name] = t
            kb.dma("sp", lambda q: q.dma_start(out=t, in_=sb_ap), rd=rd, wr=[Reg()])

        def finish():
            kb.barrier(engs=("sp",))

        xnT = sbt(pA, "xnT", [128, KC, T], BF16)
        RxnT = [R(f"xnT{tc}") for tc in range(4)]
        nmwT = sbt(pA, "nmwT", [128, KC], F32)
        kb.dma("sp", lambda q: q.dma_start(out=nmwT[:], in_=nmw_d), wr=[R("nmwT")])

        with contextlib.ExitStack() as p0:
            xt = [sbt(p0, f"xt{i}", [128, D], F32) for i in range(3)]
            xs = [sbt(p0, f"xs{i}", [128, D], BF16) for i in range(3)]
            junk = sbt(p0, "junk0", [128, D], BF16)
            ss = sbt(p0, "ss0", [128, NT], F32)
            rstd = sbt(p0, "rstd0", [128, NT], F32)
            kb.op("dve", lambda: nc.vector.memset(ss[:], 0.0), wr=[R("ss0")])
            for i in range(NT):
                b = i % 3
                kb.dma("sp", lambda q: q.dma_start(out=xt[b][:], in_=x_d[i * 128:(i + 1) * 128, :]), wr=[R(f"xt{b}")])
                kb.op("act", lambda: nc.scalar.activation(out=junk[:], in_=xt[b][:], func=AF.Square,
                                                          accum_out=ss[:, i:i + 1]),
                      rd=[R(f"xt{b}"), R("ss0")], wr=[R("junk0"), R(f"ss0_{i}")])
                kb.op("act", lambda: nc.scalar.activation(out=rstd[:, i:i + 1], in_=ss[:, i:i + 1], func=AF.Sqrt,
                                                          bias=eps_t[:, 0:1], scale=1.0 / D),
                      rd=[R(f"ss0_{i}"), R("eps_t")], wr=[R(f"rstd0_{i}")])
                kb.op("dve", lambda: nc.vector.reciprocal(out=rstd[:, i:i + 1], in_=rstd[:, i:i + 1]),
                      rd=[R(f"rstd0_{i}")], wr=[R(f"rstd0_{i}")])
                def post0(i):
                    b = i % 3
                    kb.op("act", lambda: nc.scalar.mul(out=xs[b][:], in_=xt[b][:], mul=rstd[:, i:i + 1]),
                          rd=[R(f"xt{b}"), R(f"rstd0_{i}")], wr=[R(f"xs{b}")])
                    for half in range(2):
                        bk = (2 * i + half) % 4
                        for k in range(8):
                            kc = half * 8 + k
                            kb.op("pe", lambda: nc.tensor.transpose(out=psb[bk][:, k * 128:(k + 1) * 128],
                                                                    in_=xs[b][:, kc * 128:(kc + 1) * 128], identity=identb),
                                  rd=[R(f"xs{b}"), RCB], wr=[Rps[bk]])
                        kb.op("dve", lambda: nc.vector.tensor_tensor(
                            out=xnT[:, half * 8:(half + 1) * 8, i * 128:(i + 1) * 128],
                            in0=psb[bk][:, 0:1024].rearrange("p (k t) -> p k t", t=128),
                            in1=nmwT[:, half * 8:(half + 1) * 8].unsqueeze(2).to_broadcast([128, 8, 128]),
                            op=ALU.mult), rd=[Rps[bk], R("nmwT")], wr=[RxnT[i // 4]])
                if i >= 1:
                    post0(i - 1)
            post0(NT - 1)
            kb.barrier()
        tap("xnT", xnT[:, 0:2, :], [128, 2, T], BF16, RxnT)
        if NCONV > 0:
            kb.pace = 15
            kb.tick = conv_tick
        if stop == "p0":
            finish()
            return nc, tap_out

        def proj_fm(slot, ncolchunks, evac, banks):
            for c in range(ncolchunks):
                for tc in range(4):
                    bk = banks[bank_rr["i"] % len(banks)]
                    bank_rr["i"] += 1
                    for kc in range(KC):
                        kb.op("pe", lambda: nc.tensor.matmul(ps[bk][:, :], lhsT=wring[slot][:, kc, c * 128:(c + 1) * 128],
                                                             rhs=xnT[:, kc, tc * 512:(tc + 1) * 512],
                                                             start=(kc == 0), stop=(kc == KC - 1)),
                              rd=[Rwr[slot], RxnT[tc]], wr=[Rps[bk]])
                    evac(c, tc, bk)
                    yield

        def proj_tm(slot, ncols, evac, banks, per=1):
            for i in range(NT):
                bk = banks[bank_rr["i"] % len(banks)]
                bank_rr["i"] += 1
                for kc in range(KC):
                    kb.op("pe", lambda: nc.tensor.matmul(ps[bk][:, 0:ncols], lhsT=xnT[:, kc, i * 128:(i + 1) * 128],
                                                         rhs=wring[slot][:, kc, 0:ncols],
                                                         start=(kc == 0), stop=(kc == KC - 1)),
                          rd=[Rwr[slot], RxnT[i // 4]], wr=[Rps[bk]])
                evac(i, bk)
                if (i + 1) % per == 0:
                    yield

        ev_rr = {"i": 0}

        def evac_copy(out_ap, in_ap, rd, wr, eng=None):
            e = "act" if ev_rr["i"] % 2 == 0 else "dve"
            if eng is not None:
                e = eng
            else:
                ev_rr["i"] += 1
            if e == "act":
                kb.op("act", lambda: nc.scalar.copy(out=out_ap, in_=in_ap), rd=rd, wr=wr)
            else:
                kb.op("dve", lambda: nc.vector.tensor_copy(out=out_ap, in_=in_ap), rd=rd, wr=wr)

        with contextlib.ExitStack() as p1:
            fqT = [sbt(p1, f"fqT{i}", [128, 2, T], BF16) for i in range(2)]
            fkT = [sbt(p1, f"fkT{i}", [128, 2, T], BF16) for i in range(2)]
            fvt = [sbt(p1, f"fvt{i}", [128, NT, 256], BF16) for i in range(2)]
            foT = [sbt(p1, f"foT{i}", [128, T], BF16) for i in range(2)]
            PT = [sbt(p1, f"PT{i}", [128, 512], BF16) for i in range(3)]
            rz = [sbt(p1, f"rz{i}", [128, 512], F32) for i in range(2)]
            biasT = sbt(p1, "biasT", [128, NH, NT, NT], F32)
            wf = sbt(p1, "wf", [128, KC, 8], BF16)
            bf_bc = sbt(p1, "bf_bc", [128, 128], F32)
            zt = sbt(p1, "zt", [128, 128], F32)
            spt = sbt(p1, "spt", [128, 128], F32)
            offs = sbt(p1, "offs", [128, NT, NH], F32)
            csp = sbt(p1, "csp", [128, NT, NH], F32)
            crefb = sbt(p1, "crefb", [128, NT, NH], F32)

            kb.dma("pool", lambda q: q.dma_start(
                out=wf[:], in_=w_in_d[:, C_FL:C_FL + 8].rearrange("(kc p) n -> p kc n", p=128)), wr=[R("wf")])
            kb.dma("sp", lambda q: q.dma_start(out=bf_bc[:], in_=bf_d), wr=[R("bf_bc")])
            order1 = []
            for g in range(4):
                order1 += [C_FQ + 256 * g, C_FK + 256 * g, C_FV + 256 * g]
            wq1 = {"next": 0, "slots": {}}

            def prefetch1():
                k = wq1["next"]
                if k < len(order1):
                    wq1["slots"][k] = load_w(order1[k])
                    wq1["next"] += 1

            for _ in range(NWR):
                prefetch1()

            for i in range(NT):
                for kc in range(KC):
                    kb.op("pe", lambda: nc.tensor.matmul(ps[0][:, i * 8:(i + 1) * 8], lhsT=xnT[:, kc, i * 128:(i + 1) * 128],
                                                         rhs=wf[:, kc, :], start=(kc == 0), stop=(kc == KC - 1)),
                          rd=[R("wf"), RxnT[i // 4]], wr=[Rps[0]])
            kb.op("dve", lambda: nc.vector.tensor_tensor(out=zt[:], in0=ps[0][:, 0:128], in1=bf_bc[:], op=ALU.add),
                  rd=[Rps[0], R("bf_bc")], wr=[R("zt")])
            kb.op("act", lambda: nc.scalar.activation(out=zt[:], in_=zt[:], func=AF.Exp, scale=-1.0),
                  rd=[R("zt")], wr=[R("zt")])
            kb.op("act", lambda: nc.scalar.activation(out=spt[:], in_=zt[:], func=AF.Ln, bias=one_t[:, 0:1], scale=1.0),
                  rd=[R("zt"), R("one_t")], wr=[R("spt")])
            tap("sp", spt[:], [128, 128], F32, [R("spt")])
            kb.op("pe", lambda: nc.tensor.matmul(ps[1][:, 0:128], lhsT=Ucumf, rhs=spt[:], start=True, stop=True),
                  rd=[RCF, R("spt")], wr=[Rps[1]])
            kb.op("pe", lambda: nc.tensor.matmul(ps[2][:, 0:128], lhsT=onesf, rhs=spt[:], start=True, stop=True),
                  rd=[RCF, R("spt")], wr=[Rps[2]])
            kb.op("dve", lambda: nc.vector.memset(offs[:], 0.0), wr=[R("offs")])
            for i in range(1, NT):
                kb.op("dve", lambda: nc.vector.tensor_tensor(out=offs[:, i, :], in0=offs[:, i - 1, :],
                                                             in1=ps[2][:, (i - 1) * 8:i * 8], op=ALU.add),
                      rd=[R("offs"), Rps[2]], wr=[R("offs")])
            kb.op("dve", lambda: nc.vector.tensor_tensor(out=csp[:].rearrange("p a b -> p (a b)"), in0=ps[1][:, 0:128],
                                                         in1=offs[:].rearrange("p a b -> p (a b)"), op=ALU.add),
                  rd=[R("offs"), Rps[1]], wr=[R("csp")])
            kb.op("pe", lambda: nc.tensor.matmul(ps[3][:, 0:128], lhsT=sel127f, rhs=csp[:].rearrange("p a b -> p (a b)"),
                                                 start=True, stop=True), rd=[RCF, R("csp")], wr=[Rps[3]])
            kb.op("dve", lambda: nc.vector.tensor_copy(out=crefb[:].rearrange("p a b -> p (a b)"), in_=ps[3][:, 0:128]),
                  rd=[Rps[3]], wr=[R("crefb")])
            for h in range(NH):
                kb.op("dve", lambda: nc.vector.tensor_tensor(
                    out=biasT[:, h, :, :], in0=csp[:, :, h].unsqueeze(2).to_broadcast([128, NT, NT]),
                    in1=crefb[:, :, h].unsqueeze(1).to_broadcast([128, NT, NT]), op=ALU.subtract),
                    rd=[R("csp"), R("crefb")], wr=[R("biasT")])
            tap("csp", csp[:], [128, NT, NH], F32, [R("csp")])

            def gen_proj1(g):
                b = g % 2
                for part, dst, nm in ((0, fqT, "fqT"), (1, fkT, "fkT")):
                    slot = wq1["slots"][3 * g + part]

                    def ev(c, tc, bk, dst=dst, nm=nm):
                        evac_copy(dst[b][:, c, tc * 512:(tc + 1) * 512], ps[bk][:, :], [Rps[bk]], [R(f"{nm}{b}_{tc}")])
                    yield from proj_fm(slot, 2, ev, (6, 7))
                    prefetch1()
                slot = wq1["slots"][3 * g + 2]

                def evv(i, bk):
                    evac_copy(fvt[b][:, i, :], ps[bk][:, 0:256], [Rps[bk]], [R(f"fvt{b}_{i // 4}")])
                yield from proj_tm(slot, 256, evv, (6, 7))
                prefetch1()

            scale = 1.0 / math.sqrt(HD)
            pt_rr = {"i": 0}

            deferred1 = []

            def flush_deferred1():
                while deferred1:
                    deferred1.pop(0)()

            def gen_attn(g):
                b = g % 2
                for hh in range(2):
                    h = 2 * g + hh
                    fb = h % 2
                    for qc in range(4):
                        if qc == 1:
                            flush_deferred1()
                        ob = 2 + (qc % 2)
                        zb = 4 + (qc % 2)
                        nkb = 4 * qc + 4
                        pend = None
                        for kbk in range(nkb + 1):
                            if kbk < nkb:
                                j0 = max(0, kbk - 4 * qc)
                                sbk = kbk % 2
                                c0 = j0 * 128
                                kb.op("pe", lambda: nc.tensor.matmul(
                                    ps[sbk][:, c0:512], lhsT=fkT[b][:, hh, kbk * 128:(kbk + 1) * 128],
                                    rhs=fqT[b][:, hh, qc * 512 + c0:(qc + 1) * 512], start=True, stop=True),
                                    rd=[R(f"fkT{b}_{kbk // 4}"), R(f"fqT{b}_{qc}")], wr=[Rps[sbk]])
                                pi = pt_rr["i"] % 3
                                pt_rr["i"] += 1
                                for j in range(j0, 4):
                                    qb = 4 * qc + j
                                    kb.op("act", lambda: nc.scalar.activation(
                                        out=PT[pi][:, j * 128:(j + 1) * 128], in_=ps[sbk][:, j * 128:(j + 1) * 128],
                                        func=AF.Exp, bias=biasT[:, h, kbk, qb:qb + 1], scale=scale),
                                        rd=[Rps[sbk], R("biasT")], wr=[R(f"PT{pi}")])
                                if kbk >= 4 * qc:
                                    kb.op("dve", lambda: nc.vector.tensor_tensor(
                                        out=PT[pi][:, c0:c0 + 128], in0=PT[pi][:, c0:c0 + 128], in1=maskTb, op=ALU.mult),
                                        rd=[R(f"PT{pi}"), RCB], wr=[R(f"PT{pi}")])
                                cur = (kbk, pi, c0)
                            else:
                                cur = None
                            if pend is not None:
                                pk, ppi, pc0 = pend
                                kb.op("pe", lambda: nc.tensor.matmul(
                                    ps[ob][:, pc0:512], lhsT=fvt[b][:, pk, hh * 128:(hh + 1) * 128], rhs=PT[ppi][:, pc0:512],
                                    start=(pk == 0), stop=(pk == nkb - 1)),
                                    rd=[R(f"fvt{b}_{pk // 4}"), R(f"PT{ppi}")], wr=[Rps[ob]])
                                kb.op("pe", lambda: nc.tensor.matmul(
                                    ps[zb][:, pc0:512], lhsT=onesb, rhs=PT[ppi][:, pc0:512],
                                    start=(pk == 0), stop=(pk == nkb - 1)),
                                    rd=[RCB, R(f"PT{ppi}")], wr=[Rps[zb]])
                            pend = cur
                            yield
                        rzi = qc % 2
                        kb.op("dve", lambda: nc.vector.reciprocal(out=rz[rzi][:], in_=ps[zb][:, :]),
                              rd=[Rps[zb]], wr=[R(f"rz{rzi}")])
                        kb.op("dve", lambda: nc.vector.tensor_tensor(out=foT[fb][:, qc * 512:(qc + 1) * 512],
                                                                     in0=ps[ob][:, :], in1=rz[rzi][:], op=ALU.mult),
                              rd=[Rps[ob], R(f"rz{rzi}")], wr=[R(f"foT{fb}")])
                    deferred1.append(lambda h=h, fb=fb: kb.dma(
                        "sp", lambda q: q.dma_start(out=foxo_d[h], in_=foT[fb][:]), rd=[R(f"foT{fb}")], wr=[R(f"foxo_d{h}")]))
                    if h == 0:
                        tap("foT0", foT[fb][:], [128, T], BF16, [R(f"foT{fb}")])

            for _ in gen_proj1(0):
                pass
            tap("fqT0", fqT[0][:, 0, :], [128, T], BF16, [R(f"fqT0_{tc}") for tc in range(4)])
            tap("fvt0", fvt[0][:], [128, NT, 256], BF16, [R(f"fvt0_{tc}") for tc in range(4)])
            for g in range(4):
                _interleave(gen_attn(g), gen_proj1(g + 1) if g < 3 else None, 3)
            flush_deferred1()
            kb.barrier()
        if stop == "p1":
            finish()
            return nc, tap_out

        NC2 = 2 * NH * 128 + NH + 2
        kb.pace = 18
        with contextlib.ExitStack() as p2:
            cret = sbt(p2, "cret", [128, NC2], F32)
            kb.dma("sp", lambda q: q.dma_start(out=cret[:], in_=cret_d), wr=[R("cret")])
            RCR = R("cret")
            decT = cret[:, 0:1024]
            xib = cret[:, 1024:2048]
            zeta = cret[:, 2048:2056]
            invf = cret[:, 2056:2057]
            ssign = cret[:, 2057:2058]
            gnwT = sbt(p2, "gnwT", [128, NH], F32)
            kb.dma("sp", lambda q: q.dma_start(out=gnwT[:], in_=gnw_d), wr=[R("gnwT")])
            cosT = sbt(p2, "cosT", [128, T], F32)
            sinS = sbt(p2, "sinS", [128, T], F32)
            order2 = []
            for g in range(NH):
                order2 += [C_RV + 128 * g, C_RQ + 128 * g, C_RK + 128 * g, C_RG + 128 * g]
            wq2 = {"next": 0, "slots": {}}

            def prefetch2():
                k = wq2["next"]
                if k < len(order2):
                    wq2["slots"][k] = load_w(order2[k], 128)
                    wq2["next"] += 1

            for _ in range(NWR):
                prefetch2()
            with contextlib.ExitStack() as p2a:
                posi = sbt(p2a, "posi", [128, T], I32)
                ang = sbt(p2a, "ang", [128, T], F32)
                tq = sbt(p2a, "tq", [128, T], F32)
                ki = sbt(p2a, "ki", [128, T], I32)
                kb.dma("sp", lambda q: q.dma_start(out=posi[:], in_=pos_d.partition_broadcast(128)), wr=[R("posi")])
                kb.op("dve", lambda: nc.vector.tensor_copy(out=ang[:], in_=posi[:]), rd=[R("posi")], wr=[R("ang")])
                kb.op("dve", lambda: nc.vector.tensor_scalar(out=ang[:], in0=ang[:], scalar1=invf, scalar2=None, op0=ALU.mult),
                      rd=[R("ang"), RCR], wr=[R("ang")])
                C1 = 6.28125
                C2 = 2.0 * PI - C1
                for which, dst, shift in (("sin", sinS, 0.0), ("cos", cosT, PI / 2)):
                    kb.op("dve", lambda: nc.vector.tensor_scalar(out=tq[:], in0=ang[:], scalar1=shift, scalar2=1.0 / (2 * PI),
                                                                 op0=ALU.add, op1=ALU.mult), rd=[R("ang")], wr=[R("tq")])
                    kb.op("dve", lambda: nc.vector.tensor_copy(out=ki[:], in_=tq[:]), rd=[R("tq")], wr=[R("ki")])
                    kb.op("dve", lambda: nc.vector.tensor_copy(out=tq[:], in_=ki[:]), rd=[R("ki")], wr=[R("tq")])
                    kb.op("dve", lambda: nc.vector.scalar_tensor_tensor(out=dst[:], in0=tq[:], scalar=-C1, in1=ang[:],
                                                                        op0=ALU.mult, op1=ALU.add),
                          rd=[R("tq"), R("ang")], wr=[R(which)])
                    kb.op("dve", lambda: nc.vector.scalar_tensor_tensor(out=dst[:], in0=tq[:], scalar=-C2, in1=dst[:],
                                                                        op0=ALU.mult, op1=ALU.add),
                          rd=[R("tq"), R(which)], wr=[R(which)])
                    kb.op("dve", lambda: nc.vector.tensor_scalar(out=dst[:], in0=dst[:], scalar1=shift, scalar2=-PI,
                                                                 op0=ALU.add, op1=ALU.max), rd=[R(which)], wr=[R(which)])
                    kb.op("dve", lambda: nc.vector.tensor_scalar(out=dst[:], in0=dst[:], scalar1=PI, scalar2=None,
                                                                 op0=ALU.min), rd=[R(which)], wr=[R(which)])
                    kb.op("act", lambda: nc.scalar.activation(out=dst[:], in_=dst[:], func=AF.Sin), rd=[R(which)], wr=[R(which)])
                kb.op("dve", lambda: nc.vector.tensor_scalar(out=sinS[:], in0=sinS[:], scalar1=ssign, scalar2=None, op0=ALU.mult),
                      rd=[R("sin"), RCR], wr=[R("sin")])
                kb.barrier()
            tap("cosT", cosT[:], [128, T], F32, [R("cos")])
            tap("sinS", sinS[:], [128, T], F32, [R("sin")])
            if stop == "p2a":
                kb.barrier()
                finish()
                return nc, tap_out

            rqraw = [sbt(p2, f"rqraw{i}", [128, 1, T], BF16) for i in range(2)]
            rkraw = [sbt(p2, f"rkraw{i}", [128, 1, T], BF16) for i in range(2)]
            rvt = [sbt(p2, f"rvt{i}", [128, NT, 128], BF16) for i in range(2)]
            srg = [sbt(p2, f"srg{i}", [128, 1, T], BF16) for i in range(2)]
            qrot = sbt(p2, "qrot", [128, T], BF16)
            krot = sbt(p2, "krot", [128, T], BF16)
            qxi = sbt(p2, "qxi", [128, T], BF16)
            kz = sbt(p2, "kz", [128, NT, 128], BF16)
            stb = sbt(p2, "stb", [128, NT, 128], BF16)
            st = sbt(p2, "st", [128, 128], F32)
            tA = [sbt(p2, f"tA{i}", [128, 512], F32) for i in range(2)]
            tB = [sbt(p2, f"tB{i}", [128, 512], F32) for i in range(2)]
            innb = [sbt(p2, f"innb{i}", [128, 4, 128], BF16) for i in range(2)]
            ynb = [sbt(p2, f"ynb{i}", [128, 4, 128], BF16) for i in range(2)]
            sq = sbt(p2, "sq", [128, 512], F32)
            stat = sbt(p2, "stat", [128, 6, 4], F32)
            roT = [sbt(p2, f"roT{i}", [128, T], BF16) for i in range(2)]

            def gen_proj2(g):
                b = g % 2
                slot = wq2["slots"][4 * g + 0]

                def evv(i, bk):
                    evac_copy(rvt[b][:, i, :], ps[bk][:, 0:128], [Rps[bk]], [R(f"rvt{b}_{i // 4}")], eng="act")
                yield from proj_tm(slot, 128, evv, (1, 6, 7), per=2)
                prefetch2()
                for part, dst, nm in ((1, rqraw, "rqraw"), (2, rkraw, "rkraw")):
                    slot = wq2["slots"][4 * g + part]

                    def ev(c, tc, bk, dst=dst, nm=nm):
                        evac_copy(dst[b][:, c, tc * 512:(tc + 1) * 512], ps[bk][:, :], [Rps[bk]], [R(f"{nm}{b}_{tc}")], eng="act")
                    yield from proj_fm(slot, 1, ev, (1, 6, 7))
                    prefetch2()
                slot = wq2["slots"][4 * g + 3]

                def evg(c, tc, bk):
                    kb.op("act", lambda: nc.scalar.activation(out=srg[b][:, c, tc * 512:(tc + 1) * 512], in_=ps[bk][:, :],
                                                              func=AF.Silu), rd=[Rps[bk]], wr=[R(f"srg{b}_{tc}")])
                yield from proj_fm(slot, 1, evg, (1, 6, 7))
                prefetch2()

            tmp_rr = {"i": 0}

            deferred2 = []

            def flush_deferred2():
                while deferred2:
                    deferred2.pop(0)()

            def gen_ret(g):
                b = g % 2
                for hh in range(1):
                    h = g
                    rb = h % 2
                    gch = float(np.exp(np.float32(128.0) * np.log1p(-np.float32(2.0 ** (-5.0 - h)))))
                    for tc in range(4):
                        sl = slice(tc * 512, (tc + 1) * 512)
                        for src, dst, nm, bk in ((rqraw, qrot, "qrot", 0), (rkraw, krot, "krot", 0)):
                            ti = tmp_rr["i"] % 2
                            tmp_rr["i"] += 1
                            kb.op("pe", lambda: nc.tensor.matmul(ps[bk][:, :], lhsT=Rswapb, rhs=src[b][:, hh, sl],
                                                                 start=True, stop=True),
                                  rd=[RCB, R(f"r{nm[0]}raw{b}_{tc}")], wr=[Rps[bk]])
                            kb.op("dve", lambda: nc.vector.tensor_tensor(out=tA[ti][:], in0=src[b][:, hh, sl], in1=cosT[:, sl],
                                                                         op=ALU.mult),
                                  rd=[R(f"r{nm[0]}raw{b}_{tc}"), R("cos")], wr=[R(f"tA{ti}")])
                            kb.op("dve", lambda: nc.vector.tensor_tensor(out=tB[ti][:], in0=ps[bk][:, :], in1=sinS[:, sl],
                                                                         op=ALU.mult),
                                  rd=[Rps[bk], R("sin")], wr=[R(f"tB{ti}")])
                            kb.op("dve", lambda: nc.vector.tensor_tensor(out=dst[:, sl], in0=tA[ti][:], in1=tB[ti][:], op=ALU.add),
                                  rd=[R(f"tA{ti}"), R(f"tB{ti}")], wr=[R(f"{nm}_{tc}")])
                        kb.op("dve", lambda: nc.vector.tensor_tensor(
                            out=qxi[:, sl].rearrange("p (c n) -> p c n", n=128),
                            in0=qrot[:, sl].rearrange("p (c n) -> p c n", n=128),
                            in1=xib[:, h * 128:(h + 1) * 128].unsqueeze(1).to_broadcast([128, 4, 128]), op=ALU.mult),
                            rd=[R(f"qrot_{tc}"), RCR], wr=[R(f"qxi_{tc}")])
                        yield
                    if h == 0:
                        tap("qrot0", qrot[:], [128, T], BF16, [R(f"qrot_{tc}") for tc in range(4)])
                        tap("krot0", krot[:], [128, T], BF16, [R(f"krot_{tc}") for tc in range(4)])
                    for half in range(2):
                        for k in range(8):
                            c = half * 8 + k
                            kb.op("pe", lambda: nc.tensor.transpose(out=psb[2][:, k * 128:(k + 1) * 128],
                                                                    in_=krot[:, c * 128:(c + 1) * 128], identity=identb),
                                  rd=[R(f"krot_{c // 4}"), RCB], wr=[Rps[2]])
                        kb.op("dve", lambda: nc.vector.tensor_scalar(
                            out=kz[:, half * 8:(half + 1) * 8, :], in0=psb[2][:, 0:1024].rearrange("p (k t) -> p k t", t=128),
                            scalar1=zeta[:, h:h + 1], scalar2=None, op0=ALU.mult), rd=[Rps[2], RCR], wr=[R("kz")])
                        yield
                    flush_deferred2()
                    kb.op("dve", lambda: nc.vector.memset(st[:], 0.0), wr=[R("st")])
                    for c4 in range(4):
                        for cc in range(4):
                            c = 4 * c4 + cc
                            if c == 15:
                                continue
                            kb.op("pe", lambda: nc.tensor.matmul(ps[3][:, cc * 128:(cc + 1) * 128], lhsT=kz[:, c, :],
                                                                 rhs=rvt[b][:, c, hh * 128:(hh + 1) * 128], start=True, stop=True),
                                  rd=[R("kz"), R(f"rvt{b}_{c // 4}")], wr=[Rps[3]])
                        for cc in range(4):
                            c = 4 * c4 + cc
                            if c == 15:
                                continue
                            kb.op("dve", lambda: nc.vector.scalar_tensor_tensor(
                                out=st[:], in0=st[:], scalar=gch, in1=ps[3][:, cc * 128:(cc + 1) * 128],
                                op0=ALU.mult, op1=ALU.add), rd=[R("st"), Rps[3]], wr=[R("st")])
                            kb.op("dve", lambda: nc.vector.tensor_copy(out=stb[:, c + 1, :], in_=st[:]), rd=[R("st")], wr=[R("stb")])
                        yield
                    for c4 in range(4):
                        ib = c4 % 2
                        sl = slice(c4 * 512, (c4 + 1) * 512)
                        for cc in range(4):
                            c = 4 * c4 + cc
                            cs = slice(c * 128, (c + 1) * 128)
                            kb.op("pe", lambda: nc.tensor.matmul(ps[4][:, cc * 128:(cc + 1) * 128], lhsT=krot[:, cs], rhs=qrot[:, cs],
                                                                 start=True, stop=True),
                                  rd=[R(f"krot_{c4}"), R(f"qrot_{c4}")], wr=[Rps[4]])
                        kb.op("dve", lambda: nc.vector.tensor_tensor(
                            out=innb[ib][:], in0=ps[4][:, :].rearrange("p (c n) -> p c n", n=128),
                            in1=decT[:, h * 128:(h + 1) * 128].unsqueeze(1).to_broadcast([128, 4, 128]), op=ALU.mult),
                            rd=[Rps[4], RCR], wr=[R(f"innb{ib}")])
                        yield
                        for cc in range(4):
                            c = 4 * c4 + cc
                            cs = slice(c * 128, (c + 1) * 128)
                            kb.op("pe", lambda: nc.tensor.matmul(ps[5][:, cc * 128:(cc + 1) * 128], lhsT=innb[ib][:, cc, :],
                                                                 rhs=rvt[b][:, c, hh * 128:(hh + 1) * 128],
                                                                 start=True, stop=(c == 0)),
                                  rd=[R(f"innb{ib}"), R(f"rvt{b}_{c4}")], wr=[Rps[5]])
                            if c > 0:
                                kb.op("pe", lambda: nc.tensor.matmul(ps[5][:, cc * 128:(cc + 1) * 128], lhsT=qxi[:, cs],
                                                                     rhs=stb[:, c, :], start=False, stop=True),
                                      rd=[R(f"qxi_{c4}"), R("stb")], wr=[Rps[5]])
                        if h == 0 and c4 == 0:
                            kb.op("dve", lambda: nc.vector.tensor_copy(out=tA[0][:], in_=ps[5][:, :]), rd=[Rps[5]], wr=[R("tA0")])
                            tap("retraw0", tA[0][:], [128, 512], F32, [R("tA0")])
                        if DBG == 2:
                            yield
                            continue
                        kb.op("dve", lambda: nc.vector.tensor_copy(out=tA[1][:], in_=ps[5][:, :]), rd=[Rps[5]], wr=[R("tA1")])
                        o3 = tA[1][:].rearrange("p (c n) -> p c n", n=128)
                        kb.op("dve", lambda: nc.vector.tensor_reduce(out=stat[:, 0, :], in_=o3, axis=AX.X, op=ALU.add),
                              rd=[R("tA1")], wr=[R("stat0")])
                        if DBG == 31:
                            yield
                            continue
                        kb.op("dve", lambda: nc.vector.tensor_tensor(out=sq[:], in0=tA[1][:], in1=tA[1][:], op=ALU.mult),
                              rd=[R("tA1")], wr=[R("sq")])
                        if DBG == 32:
                            yield
                            continue
                        kb.op("dve", lambda: nc.vector.tensor_reduce(out=stat[:, 1, :], in_=sq[:].rearrange("p (c n) -> p c n", n=128),
                                                                     axis=AX.X, op=ALU.add), rd=[R("sq")], wr=[R("stat1")])
                        if DBG == 3:
                            yield
                            continue
                        kb.op("dve", lambda: nc.vector.tensor_scalar(out=stat[:, 2, :], in0=stat[:, 0, :], scalar1=1.0 / 128,
                                                                     scalar2=None, op0=ALU.mult), rd=[R("stat0")], wr=[R("stat2")])
                        kb.op("dve", lambda: nc.vector.tensor_tensor(out=stat[:, 3, :], in0=stat[:, 2, :], in1=stat[:, 2, :],
                                                                     op=ALU.mult), rd=[R("stat2")], wr=[R("stat3")])
                        kb.op("dve", lambda: nc.vector.scalar_tensor_tensor(out=stat[:, 4, :], in0=stat[:, 1, :], scalar=1.0 / 128,
                                                                            in1=stat[:, 3, :], op0=ALU.mult, op1=ALU.subtract),
                              rd=[R("stat1"), R("stat3")], wr=[R("stat4")])
                        kb.op("act", lambda: nc.scalar.activation(out=stat[:, 5, :], in_=stat[:, 4, :], func=AF.Sqrt,
                                                                  bias=eps_t[:, 0:1], scale=1.0),
                              rd=[R("stat4"), R("eps_t")], wr=[R("stat5")])
                        kb.op("dve", lambda: nc.vector.reciprocal(out=stat[:, 5, :], in_=stat[:, 5, :]),
                              rd=[R("stat5")], wr=[R("stat5")])
                        if DBG == 4:
                            yield
                            continue
                        kb.op("dve", lambda: nc.vector.tensor_tensor(
                            out=tA[1][:].rearrange("p (c n) -> p c n", n=128), in0=o3,
                            in1=stat[:, 2, :].unsqueeze(2).to_broadcast([128, 4, 128]), op=ALU.subtract),
                            rd=[R("tA1"), R("stat2")], wr=[R("tA1")])
                        kb.op("dve", lambda: nc.vector.tensor_tensor(
                            out=ynb[ib][:], in0=tA[1][:].rearrange("p (c n) -> p c n", n=128),
                            in1=stat[:, 5, :].unsqueeze(2).to_broadcast([128, 4, 128]), op=ALU.mult),
                            rd=[R("tA1"), R("stat5")], wr=[R(f"ynb{ib}")])
                        yield
                        yield
                        for cc in range(4):
                            kb.op("pe", lambda: nc.tensor.transpose(out=psb[2][:, cc * 128:(cc + 1) * 128], in_=ynb[ib][:, cc, :],
                                                                    identity=identb),
                                  rd=[R(f"ynb{ib}"), RCB], wr=[Rps[2]])
                        kb.op("dve", lambda: nc.vector.scalar_tensor_tensor(
                            out=roT[rb][:, sl], in0=psb[2][:, 0:512], scalar=gnwT[:, h:h + 1], in1=srg[b][:, hh, sl],
                            op0=ALU.mult, op1=ALU.mult), rd=[Rps[2], R("gnwT"), R(f"srg{b}_{c4}")], wr=[R(f"roT{rb}")])
                        yield
                    deferred2.append(lambda h=h, rb=rb: kb.dma(
                        "sp", lambda q: q.dma_start(out=reto_d[h], in_=roT[rb][:]), rd=[R(f"roT{rb}")], wr=[R(f"reto_d{h}")]))
                    if h == 0:
                        tap("roT0", roT[rb][:], [128, T], BF16, [R(f"roT{rb}")])

            for _ in gen_proj2(0):
                pass
            if stop == "p2b":
                kb.barrier()
                finish()
                return nc, tap_out
            if stop.startswith("p2c"):
                nun = int(stop.split(":")[1]) if ":" in stop else 1000
                for iu, _ in enumerate(gen_ret(0)):
                    if iu + 1 >= nun:
                        break
                kb.barrier()
                finish()
                return nc, tap_out
            for g in range(NH):
                if g < NH - 1:
                    _interleave(gen_ret(g), gen_proj2(g + 1), 1)
                else:
                    _interleave(gen_ret(g), None, 1)
            flush_deferred2()
            kb.barrier()
        if stop == "p2":
            finish()
            return nc, tap_out

        kb.pace = 14
        with contextlib.ExitStack() as p3:
            bgT = sbt(p3, "bgT", [128, 32], F32)
            kb.dma("sp", lambda q: q.dma_start(out=bgT[:], in_=bg_d), wr=[R("bgT")])
            gst = [sbt(p3, f"gst{i}", [128, 512], BF16) for i in range(4)]
            slots3 = {}
            nxt = {"k": 0}

            def prefetch3():
                k = nxt["k"]
                if k < 16:
                    slots3[k] = load_w(C_GATE + 256 * k)
                    nxt["k"] += 1
            for _ in range(NWR):
                prefetch3()
            gs_rr = {"i": 0}
            for k in range(16):
                def evs(c, tc, bk, k=k):
                    colchunk = 2 * k + c
                    gi = gs_rr["i"] % 4
                    gs_rr["i"] += 1
                    kb.op("act", lambda: nc.scalar.activation(out=gst[gi][:], in_=ps[bk][:, :], func=AF.Sigmoid,
                                                              bias=bgT[:, colchunk:colchunk + 1], scale=1.0),
                          rd=[Rps[bk], R("bgT")], wr=[R(f"gst{gi}")])
                    kb.dma("sp", lambda q: q.dma_start(out=gates_d[colchunk * 128:(colchunk + 1) * 128, tc * 512:(tc + 1) * 512],
                                                       in_=gst[gi][:]), rd=[R(f"gst{gi}")], wr=[R(f"gates_d{colchunk}_{tc}")])
                for _ in proj_fm(slots3[k], 2, evs, (0, 1, 2, 3, 4, 5, 6, 7)):
                    pass
                prefetch3()
            kb.barrier()
        pA.close()
        if stop == "p3":
            finish()
            return nc, tap_out

        kb.pace = 24
        pB = contextlib.ExitStack()
        es.enter_context(pB)
        logits = sbt(pB, "logits", [128, NT, 36], F32)
        p4 = contextlib.ExitStack()
        es.enter_context(p4)
        mergedT = sbt(p4, "mergedT", [128, KC, T], BF16)
        with contextlib.ExitStack() as p4a:
            foA = sbt(p4a, "foA", [128, NH, T], BF16)
            roA = sbt(p4a, "roA", [128, NH, T], BF16)
            for h in range(NH):
                kb.dma("sp", lambda q: q.dma_start(out=foA[:, h, :], in_=foxo_d[h]), rd=[R(f"foxo_d{h}")], wr=[R(f"foA{h}")])
                kb.dma("sp", lambda q: q.dma_start(out=roA[:, h, :], in_=reto_d[h]), rd=[R(f"reto_d{h}")], wr=[R(f"roA{h}")])
            wbr = [sbt(p4a, f"wbr{i}", [128, 2, NH, 256], BF16) for i in range(2)]
            gt = [sbt(p4a, f"gt{i}", [128, 2, 512], BF16) for i in range(3)]
            m0 = [sbt(p4a, f"m0_{i}", [128, 512], F32) for i in range(2)]
            m1 = [sbt(p4a, f"m1_{i}", [128, 512], F32) for i in range(2)]

            def load_wbr(jp):
                s = jp % 2
                for n in range(2):
                    src = w_br_d[n][:, jp * 256:(jp + 1) * 256].rearrange("(h p) n -> p h n", p=128)
                    kb.dma("pool", lambda q: q.dma_start(out=wbr[s][:, n, :, :], in_=src), wr=[R(f"wbr{s}_{n}")])
            load_wbr(0)
            load_wbr(1)
            it = 0
            for jp in range(8):
                s = jp % 2
                for jj in range(2):
                    j = 2 * jp + jj
                    for tc in range(4):
                        gi = it % 3
                        mi = it % 2
                        it += 1
                        for n in range(2):
                            row0 = n * D + j * 128
                            kb.dma("sp", lambda q: q.dma_start(out=gt[gi][:, n, :],
                                                               in_=gates_d[row0:row0 + 128, tc * 512:(tc + 1) * 512]),
                                   rd=[R(f"gates_d{n * 16 + j}_{tc}")], wr=[R(f"gt{gi}_{n}")])
                        b0 = (2 * it) % 8
                        b1 = (2 * it + 1) % 8
                        for n, bk, src, rn in ((0, b0, foA, "foA"), (1, b1, roA, "roA")):
                            for h in range(NH):
                                kb.op("pe", lambda: nc.tensor.matmul(ps[bk][:, :], lhsT=wbr[s][:, n, h, jj * 128:(jj + 1) * 128],
                                                                     rhs=src[:, h, tc * 512:(tc + 1) * 512],
                                                                     start=(h == 0), stop=(h == NH - 1)),
                                      rd=[R(f"wbr{s}_{n}"), R(f"{rn}{h}")], wr=[Rps[bk]])
                        kb.op("dve", lambda: nc.vector.tensor_tensor(out=m0[mi][:], in0=ps[b0][:, :], in1=gt[gi][:, 0, :], op=ALU.mult),
                              rd=[Rps[b0], R(f"gt{gi}_0")], wr=[R(f"m0_{mi}")])
                        kb.op("dve", lambda: nc.vector.tensor_tensor(out=m1[mi][:], in0=ps[b1][:, :], in1=gt[gi][:, 1, :], op=ALU.mult),
                              rd=[Rps[b1], R(f"gt{gi}_1")], wr=[R(f"m1_{mi}")])
                        kb.op("dve", lambda: nc.vector.tensor_tensor(out=mergedT[:, j, tc * 512:(tc + 1) * 512], in0=m0[mi][:],
                                                                     in1=m1[mi][:], op=ALU.add),
                              rd=[R(f"m0_{mi}"), R(f"m1_{mi}")], wr=[R(f"mergedT{tc}")])
                if jp + 2 < 8:
                    load_wbr(jp + 2)
            kb.barrier()
        tap("mergedT", mergedT[:, 0:2, :], [128, 2, T], BF16, [R(f"mergedT{tc}") for tc in range(4)])
        if stop == "p4a":
            finish()
            return nc, tap_out

        kb.pace = 19
        with contextlib.ExitStack() as p4b:
            wout = sbt(p4b, "wout", [128, KC, D], BF16)
            for cg in range(4):
                kb.dma("pool", lambda q: q.dma_start(
                    out=wout[:, :, cg * 512:(cg + 1) * 512],
                    in_=w_out_d[:, cg * 512:(cg + 1) * 512].rearrange("(kc p) n -> p kc n", p=128)), wr=[R(f"wout{cg}")])
            wr_s = sbt(p4b, "wr_s", [128, KC, 36], BF16)
            kb.dma("pool", lambda q: q.dma_start(out=wr_s[:], in_=wr_d.rearrange("(kc p) n -> p kc n", p=128)), wr=[R("wr_s")])
            nfw = sbt(p4b, "nfw", [128, D], F32)
            kb.dma("sp", lambda q: q.dma_start(out=nfw[:], in_=nfw_d), wr=[R("nfw")])
            rbias = sbt(p4b, "rbias", [128, 36], F32)
            kb.dma("sp", lambda q: q.dma_start(out=rbias[:], in_=rb_d), wr=[R("rbias")])
            xt = [sbt(p4b, f"xt4_{i}", [128, D], F32) for i in range(2)]
            ht = [sbt(p4b, f"ht{i}", [128, D], F32) for i in range(2)]
            hnb = [sbt(p4b, f"hnb{i}", [128, D], BF16) for i in range(2)]
            hnT = [sbt(p4b, "hnT0", [128, KC, 128], BF16)] * 2
            junk = sbt(p4b, "junk4", [128, D], BF16)
            ss = sbt(p4b, "ss4", [128, NT], F32)
            rstd = sbt(p4b, "rstd4", [128, NT], F32)
            kb.op("dve", lambda: nc.vector.memset(ss[:], 0.0), wr=[R("ss4")])
            def post4a(i):
                b = i % 2
                for half in range(2):
                    bk = 4 + half
                    for k in range(8):
                        kc = half * 8 + k
                        kb.op("pe", lambda: nc.tensor.transpose(out=psb[bk][:, k * 128:(k + 1) * 128],
                                                                in_=hnb[b][:, kc * 128:(kc + 1) * 128], identity=identb),
                              rd=[R(f"hnb{b}"), RCB], wr=[Rps[bk]])
                    evac_copy(hnT[b][:, half * 8:(half + 1) * 8, :], psb[bk][:, 0:1024].rearrange("p (k t) -> p k t", t=128),
                              [Rps[bk]], [R("hnT")], eng="dve")

            def post4b(i):
                b = i % 2
                for kc in range(KC):
                    kb.op("pe", lambda: nc.tensor.matmul(ps[6][:, 0:36], lhsT=hnT[b][:, kc, :], rhs=wr_s[:, kc, :],
                                                         start=(kc == 0), stop=(kc == KC - 1)),
                          rd=[R("hnT"), R("wr_s")], wr=[Rps[6]])
                kb.op("dve", lambda: nc.vector.tensor_tensor(out=logits[:, i, :], in0=ps[6][:, 0:36], in1=rbias[:], op=ALU.add),
                      rd=[Rps[6], R("rbias")], wr=[R("logits")])

            def mm4(i, cg):
                b = i % 2
                for kc in range(KC):
                    kb.op("pe", lambda: nc.tensor.matmul(ps[cg][:, :], lhsT=mergedT[:, kc, i * 128:(i + 1) * 128],
                                                         rhs=wout[:, kc, cg * 512:(cg + 1) * 512],
                                                         start=(kc == 0), stop=(kc == KC - 1)),
                          rd=[R(f"mergedT{i // 4}"), R(f"wout{cg}")], wr=[Rps[cg]])
                kb.op("dve", lambda: nc.vector.tensor_tensor(out=ht[b][:, cg * 512:(cg + 1) * 512], in0=ps[cg][:, :],
                                                             in1=xt[b][:, cg * 512:(cg + 1) * 512], op=ALU.add),
                      rd=[Rps[cg], R(f"xt4_{b}")], wr=[R(f"ht{b}")])

            kb.dma("sp", lambda q: q.dma_start(out=xt[0][:], in_=x_d[0:128, :]), wr=[R("xt4_0")])
            kb.dma("sp", lambda q: q.dma_start(out=xt[1][:], in_=x_d[128:256, :]), wr=[R("xt4_1")])
            for i in range(NT + 1):
                b = i % 2
                if i < NT:
                    mm4(i, 0)
                    mm4(i, 1)
                if i >= 1:
                    post4a(i - 1)
                if i < NT:
                    mm4(i, 2)
                    mm4(i, 3)
                if i >= 1:
                    post4b(i - 1)
                if i == NT:
                    break
                if i + 2 < NT:
                    kb.dma("sp", lambda q: q.dma_start(out=xt[b][:], in_=x_d[(i + 2) * 128:(i + 3) * 128, :]), wr=[R(f"xt4_{b}")])
                kb.dma("sp", lambda q: q.dma_start(out=h_d[i * 128:(i + 1) * 128, :], in_=ht[b][:]), rd=[R(f"ht{b}")],
                       wr=[R(f"h_d{i}")])
                kb.op("act", lambda: nc.scalar.activation(out=junk[:], in_=ht[b][:], func=AF.Square, accum_out=ss[:, i:i + 1]),
                      rd=[R(f"ht{b}"), R("ss4")], wr=[R("junk4"), R(f"ss4_{i}")])
                kb.op("act", lambda: nc.scalar.activation(out=rstd[:, i:i + 1], in_=ss[:, i:i + 1], func=AF.Sqrt,
                                                          bias=eps_t[:, 0:1], scale=1.0 / D),
                      rd=[R(f"ss4_{i}"), R("eps_t")], wr=[R(f"rstd4_{i}")])
                kb.op("dve", lambda: nc.vector.reciprocal(out=rstd[:, i:i + 1], in_=rstd[:, i:i + 1]),
                      rd=[R(f"rstd4_{i}")], wr=[R(f"rstd4_{i}")])
                kb.op("dve", lambda: nc.vector.scalar_tensor_tensor(out=hnb[b][:], in0=ht[b][:], scalar=rstd[:, i:i + 1],
                                                                    in1=nfw[:], op0=ALU.mult, op1=ALU.mult),
                      rd=[R(f"ht{b}"), R(f"rstd4_{i}"), R("nfw")], wr=[R(f"hnb{b}")])
                kb.dma("sp", lambda q: q.dma_start(out=hn_d[i * 128:(i + 1) * 128, :], in_=hnb[b][:]), rd=[R(f"hnb{b}")],
                       wr=[R(f"hn_d{i}")])
            kb.barrier()
        p4.close()
        tap("logits", logits[:], [128, NT, 36], F32, [R("logits")])
        if stop == "p4":
            finish()
            return nc, tap_out

        wt = sbt(pB, "wt", [128, 2, NT], F32)
        sloti = sbt(pB, "sloti", [128, 2, NT], I32)
        NST = CAP // 128
        p6 = contextlib.ExitStack()
        es.enter_context(p6)
        w1s = [sbt(p6, f"w1s{i}", [128, KC, DE], BF16) for i in range(2)]
        w3s = [sbt(p6, f"w3s{i}", [128, KC, DE], BF16) for i in range(2)]
        w2s = [sbt(p6, f"w2s{i}", [128, 4, D], BF16) for i in range(2)]
        xg = [sbt(p6, f"xg{i}", [128, NST, D], BF16) for i in range(2)]
        xgT = [sbt(p6, f"xgT{i}", [128, KC, CAP], BF16) for i in range(2)]
        sil = [sbt(p6, f"sil{i}", [128, CAP], F32) for i in range(2)]
        gT = [sbt(p6, f"gT{i}", [128, 4, CAP], BF16) for i in range(2)]
        ybuf = [sbt(p6, f"ybuf{i}", [128, D], BF16) for i in range(2)]

        kb.tick = None
        conv_flush()

        def load_ew(e):
            s = e % 2
            if e < NCONV:
                for mat, wd, ws, nm in (("w1", wb1_d, w1s, "w1s"), ("w3", wb3_d, w3s, "w3s"), ("w2", wb2_d, w2s, "w2s")):
                    kb.dma("pool", lambda q: q.dma_start(out=ws[s][:].rearrange("p a n -> p (a n)"), in_=wd[e]),
                           rd=[R(f"wbu_{e}_{mat}_{q8}") for q8 in range(8)], wr=[R(f"{nm}{s}")])
                return
            kb.dma("pool", lambda q: q.dma_start(out=w1s[s][:], in_=w1_d[e].rearrange("(kc p) n -> p kc n", p=128)), wr=[R(f"w1s{s}")])
            kb.dma("pool", lambda q: q.dma_start(out=w3s[s][:], in_=w3_d[e].rearrange("(kc p) n -> p kc n", p=128)), wr=[R(f"w3s{s}")])
            kb.dma("pool", lambda q: q.dma_start(out=w2s[s][:], in_=w2_d[e].rearrange("(kc p) n -> p kc n", p=128)), wr=[R(f"w2s{s}")])

        def load_xg(e):
            s = e % 2
            kb.dma("sp", lambda q: q.dma_start(out=xg[s][:], in_=xg_d[e * CAP:(e + 1) * CAP, :].rearrange("(a p) n -> p a n", p=128)),
                   rd=[R(f"xg_d_{i}_{k}") for i in range(NT) for k in range(2)], wr=[R(f"xg{s}")])
        load_ew(0)
        load_ew(1)
        with contextlib.ExitStack() as p5:
            def t5(name, shape, dt=F32):
                return sbt(p5, name, shape, dt)
            gl = logits[:, :, 0:4]
            el = logits[:, :, 4:36]
            gmax = t5("gmax", [128, NT])
            gsh = t5("gsh", [128, NT, 4])
            gsum = t5("gsum", [128, NT])
            pgrp = t5("pgrp", [128, NT])
            ohg = t5("ohg", [128, NT, 4])
            em = t5("em", [128, NT, 32])
            m1 = t5("m1", [128, NT])
            m2 = t5("m2", [128, NT])
            A1 = t5("A1", [128, NT, 32])
            A2 = t5("A2", [128, NT, 32])
            e2 = t5("e2", [128, NT, 32])
            Ab = t5("Ab", [128, NT, 32], BF16)
            cnt = t5("cnt", [128, NT, 32])
            offs5 = t5("offs5", [128, NT, 32])
            ebase = t5("ebase", [128, NT, 32])
            tmp5 = t5("tmp5", [128, NT, 32])
            w1t = t5("w1t", [128, NT])
            cs = t5("cs", [128, 2, NT])
            eb = t5("eb", [128, 2, NT])
            okk = t5("okk", [128, 2, NT])
            slf = t5("slf", [128, 2, NT])
            kb.dma("sp", lambda q: q.dma_start(out=ebase[:].rearrange("p a b -> p (a b)"), in_=ebase_d), wr=[R("ebase")])

            def V(fn, rd, wr):
                kb.op("dve", fn, rd=[R(n) for n in rd], wr=[R(n) for n in wr])

            def bc2(ap2, n):
                return ap2.unsqueeze(2).to_broadcast([128, NT, n])
            V(lambda: nc.vector.tensor_reduce(out=gmax[:], in_=gl, axis=AX.X, op=ALU.max), ["logits"], ["gmax"])
            V(lambda: nc.vector.tensor_tensor(out=gsh[:], in0=gl, in1=bc2(gmax[:], 4), op=ALU.subtract), ["logits", "gmax"], ["gsh"])
            V(lambda: nc.vector.tensor_tensor(out=ohg[:], in0=gl, in1=bc2(gmax[:], 4), op=ALU.is_equal), ["logits", "gmax"], ["ohg"])
            kb.op("act", lambda: nc.scalar.activation(out=gsh[:], in_=gsh[:], func=AF.Exp), rd=[R("gsh")], wr=[R("gsh")])
            V(lambda: nc.vector.tensor_reduce(out=gsum[:], in_=gsh[:], axis=AX.X, op=ALU.add), ["gsh"], ["gsum"])
            V(lambda: nc.vector.reciprocal(out=pgrp[:], in_=gsum[:]), ["gsum"], ["pgrp"])
            V(lambda: nc.vector.tensor_scalar(out=ohg[:], in0=ohg[:], scalar1=1e30, scalar2=-1e30, op0=ALU.mult, op1=ALU.add),
              ["ohg"], ["ohg"])
            V(lambda: nc.vector.tensor_tensor(out=em[:].rearrange("p t (g e) -> p t g e", e=8),
                                              in0=el.rearrange("p t (g e) -> p t g e", e=8),
                                              in1=ohg[:].unsqueeze(3).to_broadcast([128, NT, 4, 8]),
                                              op=ALU.add), ["logits", "ohg"], ["em"])
            V(lambda: nc.vector.tensor_reduce(out=m1[:], in_=em[:], axis=AX.X, op=ALU.max), ["em"], ["m1"])
            V(lambda: nc.vector.tensor_tensor(out=A1[:], in0=em[:], in1=bc2(m1[:], 32), op=ALU.is_equal), ["em", "m1"], ["A1"])
            V(lambda: nc.vector.scalar_tensor_tensor(out=e2[:], in0=A1[:], scalar=-1e30, in1=em[:], op0=ALU.mult, op1=ALU.add),
              ["A1", "em"], ["e2"])
            V(lambda: nc.vector.tensor_reduce(out=m2[:], in_=e2[:], axis=AX.X, op=ALU.max), ["e2"], ["m2"])
            V(lambda: nc.vector.tensor_tensor(out=A2[:], in0=e2[:], in1=bc2(m2[:], 32), op=ALU.is_equal), ["e2", "m2"], ["A2"])
            V(lambda: nc.vector.tensor_tensor(out=w1t[:], in0=m2[:], in1=m1[:], op=ALU.subtract), ["m1", "m2"], ["w1t"])
            kb.op("act", lambda: nc.scalar.activation(out=w1t[:], in_=w1t[:], func=AF.Exp), rd=[R("w1t")], wr=[R("w1t")])
            V(lambda: nc.vector.tensor_scalar(out=w1t[:], in0=w1t[:], scalar1=1.0, scalar2=None, op0=ALU.add), ["w1t"], ["w1t"])
            V(lambda: nc.vector.reciprocal(out=w1t[:], in_=w1t[:]), ["w1t"], ["w1t"])
            V(lambda: nc.vector.tensor_tensor(out=wt[:, 0, :], in0=w1t[:], in1=pgrp[:], op=ALU.mult), ["w1t", "pgrp"], ["wt0"])
            V(lambda: nc.vector.tensor_tensor(out=wt[:, 1, :], in0=pgrp[:], in1=wt[:, 0, :], op=ALU.subtract), ["pgrp", "wt0"], ["wt1"])
            V(lambda: nc.vector.tensor_tensor(out=tmp5[:], in0=A1[:], in1=A2[:], op=ALU.add), ["A1", "A2"], ["tmp5"])
            V(lambda: nc.vector.tensor_copy(out=Ab[:], in_=tmp5[:]), ["tmp5"], ["Ab"])
            Abf = Ab[:].rearrange("p a b -> p (a b)")
            kb.op("pe", lambda: nc.tensor.matmul(ps[0][:, :], lhsT=Lstrb, rhs=Abf, start=True, stop=True), rd=[RCB, R("Ab")], wr=[Rps[0]])
            kb.op("pe", lambda: nc.tensor.matmul(ps[1][:, :], lhsT=onesb, rhs=Abf, start=True, stop=True), rd=[RCB, R("Ab")], wr=[Rps[1]])
            V(lambda: nc.vector.memset(offs5[:], 0.0), [], ["offs5"])
            for i in range(1, NT):
                kb.op("dve", lambda: nc.vector.tensor_tensor(out=offs5[:, i, :], in0=offs5[:, i - 1, :],
                                                             in1=ps[1][:, (i - 1) * 32:i * 32], op=ALU.add),
                      rd=[R("offs5"), Rps[1]], wr=[R("offs5")])
            kb.op("dve", lambda: nc.vector.tensor_tensor(out=cnt[:].rearrange("p a b -> p (a b)"), in0=ps[0][:, :],
                                                         in1=offs5[:].rearrange("p a b -> p (a b)"), op=ALU.add),
                  rd=[Rps[0], R("offs5")], wr=[R("cnt")])
            for k, Ak, nm in ((0, A1, "A1"), (1, A2, "A2")):
                V(lambda: nc.vector.tensor_tensor(out=tmp5[:], in0=Ak[:], in1=cnt[:], op=ALU.mult), [nm, "cnt"], ["tmp5"])
                V(lambda: nc.vector.tensor_reduce(out=cs[:, k, :], in_=tmp5[:], axis=AX.X, op=ALU.add), ["tmp5"], [f"cs{k}"])
                V(lambda: nc.vector.tensor_tensor(out=tmp5[:], in0=Ak[:], in1=ebase[:], op=ALU.mult), [nm, "ebase"], ["tmp5"])
                V(lambda: nc.vector.tensor_reduce(out=eb[:, k, :], in_=tmp5[:], axis=AX.X, op=ALU.add), ["tmp5"], [f"eb{k}"])
                V(lambda: nc.vector.tensor_scalar(out=okk[:, k, :], in0=cs[:, k, :], scalar1=float(CAP), scalar2=None, op0=ALU.is_lt),
                  [f"cs{k}"], [f"ok{k}"])
                V(lambda: nc.vector.tensor_tensor(out=slf[:, k, :], in0=cs[:, k, :], in1=eb[:, k, :], op=ALU.add),
                  [f"cs{k}", f"eb{k}"], [f"slf{k}"])
                V(lambda: nc.vector.tensor_scalar(out=slf[:, k, :], in0=slf[:, k, :], scalar1=-float(NSLOT), scalar2=None, op0=ALU.add),
                  [f"slf{k}"], [f"slf{k}"])
                V(lambda: nc.vector.tensor_tensor(out=slf[:, k, :], in0=slf[:, k, :], in1=okk[:, k, :], op=ALU.mult),
                  [f"slf{k}", f"ok{k}"], [f"slf{k}"])
                V(lambda: nc.vector.tensor_scalar(out=slf[:, k, :], in0=slf[:, k, :], scalar1=float(NSLOT), scalar2=None, op0=ALU.add),
                  [f"slf{k}"], [f"slf{k}"])
                V(lambda: nc.vector.tensor_copy(out=sloti[:, k, :], in_=slf[:, k, :]), [f"slf{k}"], [f"sloti{k}"])
                V(lambda: nc.vector.tensor_tensor(out=wt[:, k, :], in0=wt[:, k, :], in1=okk[:, k, :], op=ALU.mult),
                  [f"wt{k}", f"ok{k}"], [f"wt{k}"])
            tap("wt", wt[:], [128, 2, NT], F32, [R("wt0"), R("wt1")])
            tap("sloti", sloti[:], [128, 2, NT], I32, [R("sloti0"), R("sloti1")])
            zrow = t5("zrow", [128, D], BF16)
            V(lambda: nc.vector.memset(zrow[:], 0.0), [], ["zrow"])
            kb.dma("sp", lambda q: q.dma_start(out=y_d[NSLOT:NSLOT + 128, :], in_=zrow[:]), rd=[R("zrow")], wr=[R("y_dz")])
            hsc = [t5(f"hsc{i}", [128, D], BF16) for i in range(4)]
            for i in range(NT):
                b = i % 4
                kb.dma("sp", lambda q: q.dma_start(out=hsc[b][:], in_=hn_d[i * 128:(i + 1) * 128, :]), rd=[R(f"hn_d{i}")],
                       wr=[R(f"hsc{b}")])
                for k in range(2):
                    kb.dma("pool", lambda q: q.indirect_dma_start(
                        out=xg_d, out_offset=bass.IndirectOffsetOnAxis(ap=sloti[:, k, i:i + 1], axis=0),
                        in_=hsc[b][:, :], in_offset=None), rd=[R(f"hsc{b}"), R(f"sloti{k}")], wr=[R(f"xg_d_{i}_{k}")])
            if stop == "p5":
                kb.barrier()
        if stop == "p5":
            finish()
            return nc, tap_out

        if True:
            load_xg(0)
            yb_rr = 0
            for e in range(NE):
                s = e % 2
                if e + 1 < NE:
                    load_xg(e + 1)
                for a in range(NST):
                    for half in range(2):
                        bk = 6 + half
                        for k in range(8):
                            kc = half * 8 + k
                            kb.op("pe", lambda: nc.tensor.transpose(out=psb[bk][:, k * 128:(k + 1) * 128],
                                                                    in_=xg[s][:, a, kc * 128:(kc + 1) * 128], identity=identb),
                                  rd=[R(f"xg{s}"), RCB], wr=[Rps[bk]])
                        evac_copy(xgT[s][:, half * 8:(half + 1) * 8, a * 128:(a + 1) * 128],
                                  psb[bk][:, 0:1024].rearrange("p (k t) -> p k t", t=128), [Rps[bk]], [R(f"xgT{s}")], eng="dve")
                for m in range(4):
                    bk = m % 2
                    for kc in range(KC):
                        kb.op("pe", lambda: nc.tensor.matmul(ps[bk][:, 0:CAP], lhsT=w1s[s][:, kc, m * 128:(m + 1) * 128],
                                                             rhs=xgT[s][:, kc, :], start=(kc == 0), stop=(kc == KC - 1)),
                              rd=[R(f"w1s{s}"), R(f"xgT{s}")], wr=[Rps[bk]])
                    bk3 = 2 + bk
                    for kc in range(KC):
                        kb.op("pe", lambda: nc.tensor.matmul(ps[bk3][:, 0:CAP], lhsT=w3s[s][:, kc, m * 128:(m + 1) * 128],
                                                             rhs=xgT[s][:, kc, :], start=(kc == 0), stop=(kc == KC - 1)),
                              rd=[R(f"w3s{s}"), R(f"xgT{s}")], wr=[Rps[bk3]])
                    kb.op("act", lambda: nc.scalar.activation(out=sil[bk][:], in_=ps[bk][:, 0:CAP], func=AF.Silu),
                          rd=[Rps[bk]], wr=[R(f"sil{bk}")])
                    kb.op("dve", lambda: nc.vector.tensor_tensor(out=gT[s][:, m, :], in0=ps[bk3][:, 0:CAP], in1=sil[bk][:],
                                                                 op=ALU.mult), rd=[Rps[bk3], R(f"sil{bk}")], wr=[R(f"gT{s}")])
                for a in range(NST):
                    yb = yb_rr % 2
                    yb_rr += 1
                    for cg in range(4):
                        bk = 4 + (cg % 2)
                        for m in range(4):
                            kb.op("pe", lambda: nc.tensor.matmul(ps[bk][:, :], lhsT=gT[s][:, m, a * 128:(a + 1) * 128],
                                                                 rhs=w2s[s][:, m, cg * 512:(cg + 1) * 512],
                                                                 start=(m == 0), stop=(m == 3)),
                                  rd=[R(f"gT{s}"), R(f"w2s{s}")], wr=[Rps[bk]])
                        evac_copy(ybuf[yb][:, cg * 512:(cg + 1) * 512], ps[bk][:, :], [Rps[bk]], [R(f"ybuf{yb}")])
                    r0 = e * CAP + a * 128
                    kb.dma("sp", lambda q: q.dma_start(out=y_d[r0:r0 + 128, :], in_=ybuf[yb][:]), rd=[R(f"ybuf{yb}")], wr=[R(f"y_d_{e}_{a}")])
                if e + 2 < NE:
                    load_ew(e + 2)
            kb.barrier()
        p6.close()
        if stop == "p6":
            finish()
            return nc, tap_out

        with contextlib.ExitStack() as p7:
            nfin = sbt(p7, "nfin", [128, D], F32)
            kb.dma("sp", lambda q: q.dma_start(out=nfin[:], in_=nfin_d), wr=[R("nfin")])
            NB7 = 3
            hb = [sbt(p7, f"hb{i}", [128, D], F32) for i in range(NB7)]
            ob = [sbt(p7, f"ob{i}", [128, D], F32) for i in range(NB7)]
            y1 = [sbt(p7, f"y1_{i}", [128, D], BF16) for i in range(NT)]
            y2 = [sbt(p7, f"y2_{i}", [128, D], BF16) for i in range(NT)]
            for i in range(NT):
                for k, yk, nm in ((0, y1, "y1_"), (1, y2, "y2_")):
                    kb.dma("pool", lambda q: q.indirect_dma_start(
                        out=yk[i][:, :], out_offset=None, in_=y_d,
                        in_offset=bass.IndirectOffsetOnAxis(ap=sloti[:, k, i:i + 1], axis=0)),
                        rd=[R(f"y_d_{e}_{a}") for e in range(NE) for a in range(NST)] + [R("y_dz"), R(f"sloti{k}")],
                        wr=[R(f"{nm}{i}")])
            junk = sbt(p7, "junk7", [128, D], BF16)
            ss = sbt(p7, "ss7", [128, NT], F32)
            rstd = sbt(p7, "rstd7", [128, NT], F32)
            kb.op("dve", lambda: nc.vector.memset(ss[:], 0.0), wr=[R("ss7")])
            for i in range(NB7):
                kb.dma("sp", lambda q: q.dma_start(out=hb[i][:], in_=h_d[i * 128:(i + 1) * 128, :]), rd=[R(f"h_d{i}")], wr=[R(f"hb{i}")])
            for i in range(NT):
                b = i % NB7
                kb.op("dve", lambda: nc.vector.scalar_tensor_tensor(out=hb[b][:], in0=y1[i][:], scalar=wt[:, 0, i:i + 1], in1=hb[b][:],
                                                                    op0=ALU.mult, op1=ALU.add),
                      rd=[R(f"y1_{i}"), R("wt0"), R(f"hb{b}")], wr=[R(f"hb{b}")])
                kb.op("dve", lambda: nc.vector.scalar_tensor_tensor(out=hb[b][:], in0=y2[i][:], scalar=wt[:, 1, i:i + 1], in1=hb[b][:],
                                                                    op0=ALU.mult, op1=ALU.add),
                      rd=[R(f"y2_{i}"), R("wt1"), R(f"hb{b}")], wr=[R(f"hb{b}")])
                kb.op("act", lambda: nc.scalar.activation(out=junk[:], in_=hb[b][:], func=AF.Square, accum_out=ss[:, i:i + 1]),
                      rd=[R(f"hb{b}"), R("ss7")], wr=[R("junk7"), R(f"ss7_{i}")])
                kb.op("act", lambda: nc.scalar.activation(out=rstd[:, i:i + 1], in_=ss[:, i:i + 1], func=AF.Sqrt,
                                                          bias=eps_t[:, 0:1], scale=1.0 / D),
                      rd=[R(f"ss7_{i}"), R("eps_t")], wr=[R(f"rstd7_{i}")])
                kb.op("dve", lambda: nc.vector.reciprocal(out=rstd[:, i:i + 1], in_=rstd[:, i:i + 1]),
                      rd=[R(f"rstd7_{i}")], wr=[R(f"rstd7_{i}")])
                kb.op("dve", lambda: nc.vector.scalar_tensor_tensor(out=ob[b][:], in0=hb[b][:], scalar=rstd[:, i:i + 1], in1=nfin[:],
                                                                    op0=ALU.mult, op1=ALU.mult),
                      rd=[R(f"hb{b}"), R(f"rstd7_{i}"), R("nfin")], wr=[R(f"ob{b}")])
                if i + NB7 < NT:
                    j = i + NB7
                    kb.dma("sp", lambda q: q.dma_start(out=hb[b][:], in_=h_d[j * 128:(j + 1) * 128, :]), rd=[R(f"h_d{j}")], wr=[R(f"hb{b}")])
                kb.dma("sp", lambda q: q.dma_start(out=out_d[i * 128:(i + 1) * 128, :], in_=ob[b][:]), rd=[R(f"ob{b}")], wr=[Reg()])
            finish()
    return nc, tap_out


def host_consts():
    f32 = np.float32
    idx = np.arange(128)
    ident = np.eye(128, dtype=f32)
    maskT = (idx[:, None] <= idx[None, :]).astype(f32)
    lstr = (idx[:, None] < idx[None, :]).astype(f32)
    ones = np.ones((128, 128), f32)
    rswap = np.zeros((128, 128), f32)
    rswap[idx, (idx + 64) % 128] = 1.0
    sel127 = np.zeros((128, 128), f32)
    sel127[127, :] = 1.0
    cmat = np.concatenate([ident, maskT, lstr, ones, rswap, sel127], axis=1)
    hh = np.arange(NH, dtype=f32)
    log_gamma = np.log1p(-(f32(2.0) ** (-5.0 - hh))).astype(f32)
    n = np.arange(128, dtype=f32)
    diff = n[None, :] - n[:, None]
    isd = f32(1.0 / math.sqrt(HD))
    decT = np.where(diff[None] >= 0, np.exp(diff[None] * log_gamma[:, None, None]), 0.0).astype(f32) * isd
    xi = (np.exp((n[None, :] + 1.0) * log_gamma[:, None]).astype(f32) * isd)
    zeta = np.exp((128.0 - 1.0 - n[None, :]) * log_gamma[:, None]).astype(f32)
    half = 64
    inv_freq = (f32(10000.0) ** (-np.arange(half, dtype=f32) / half)).astype(f32)
    invf = np.concatenate([inv_freq, inv_freq])[:, None]
    ssign = np.concatenate([-np.ones(64, f32), np.ones(64, f32)])[:, None]
    cret = np.concatenate([
        decT.transpose(1, 0, 2).reshape(128, NH * 128),
        np.broadcast_to(xi.reshape(1, NH * 128), (128, NH * 128)),
        zeta.T, invf, ssign], axis=1).astype(f32)
    ebase = np.broadcast_to((np.arange(NE, dtype=f32) * CAP)[None, None, :], (128, NT, NE)).reshape(128, NT * NE)
    return dict(cmat=np.ascontiguousarray(cmat), cret=np.ascontiguousarray(cret), ebase=np.ascontiguousarray(ebase))


def make_in_maps(inputs, cores):
    f32 = np.float32
    c = host_consts()
    g = lambda k: np.asarray(inputs[k])
    shared = dict(
        w_in=np.ascontiguousarray(g("w_in")[0], dtype=f32),
        w_branch=np.ascontiguousarray(g("w_branch")[0], dtype=f32),
        w_out=np.ascontiguousarray(g("w_out")[0], dtype=f32),
        w_router=np.ascontiguousarray(np.concatenate([g("w_router_group")[0], g("w_router_expert")[0]], axis=1), dtype=f32),
        w1=np.ascontiguousarray(g("w1")[0], dtype=f32),
        w3=np.ascontiguousarray(g("w3")[0], dtype=f32),
        w2=np.ascontiguousarray(g("w2")[0], dtype=f32),
        nmwT=np.ascontiguousarray(g("norm_mix_w")[0].reshape(KC, 128).T, dtype=f32),
        bf_bc=np.ascontiguousarray(np.broadcast_to(np.tile(g("fox_b_f")[0], NT)[None, :], (128, 128)), dtype=f32),
        gnwT=np.ascontiguousarray(g("ret_gn_w")[0].reshape(NH, 128).T, dtype=f32),
        bgT=np.ascontiguousarray(g("b_gate")[0].reshape(32, 128).T, dtype=f32),
        nfw_bc=np.ascontiguousarray(np.broadcast_to(g("norm_ffn_w")[0][None, :], (128, D)), dtype=f32),
        nfin_bc=np.ascontiguousarray(np.broadcast_to(g("norm_final_w")[None, :], (128, D)), dtype=f32),
        rbias_bc=np.ascontiguousarray(np.broadcast_to(
            np.concatenate([g("b_router_group")[0], g("b_router_expert")[0]])[None, :], (128, 36)), dtype=f32),
        **c,
    )
    maps = []
    for b in cores:
        m = dict(shared)
        m["x"] = np.ascontiguousarray(g("x")[b], dtype=f32)
        m["pos"] = np.ascontiguousarray(g("positions")[b][None, :], dtype=np.int32)
        maps.append(m)
    return maps


_CACHE = {}


def kernel(**inputs):
    if "nc" not in _CACHE:
        _CACHE["nc"] = build()[0]
    nc = _CACHE["nc"]
    in_maps = make_in_maps(inputs, list(range(8)))
    res = run_bass_kernel_spmd(nc, in_maps, core_ids=list(range(8)))
    return np.stack([np.asarray(r["out"], dtype=np.float32) for r in res.results], axis=0)
```

```python
import contextlib
import math
import os
import numpy as np
import concourse.bass as bass
import concourse.mybir as mybir
from concourse.bass_utils import run_bass_kernel_spmd

F32 = mybir.dt.float32
BF16 = mybir.dt.bfloat16
I32 = mybir.dt.int32
AF = mybir.ActivationFunctionType
ALU = mybir.AluOpType
AX = mybir.AxisListType

T = 2048
D = 2048
NT = 16
KC = 16
HD = 128
NH = 8
IN_W = 11272
C_FQ, C_FK, C_FV, C_FL = 0, 1024, 2048, 3072
C_RQ, C_RK, C_RV, C_RG, C_GATE = 3080, 4104, 5128, 6152, 7176
NE = 32
DE = 512
CAP = 256
NSLOT = NE * CAP
EPS = 1e-6
PI = math.pi


class Reg:
    __slots__ = ("w", "r")

    def __init__(self):
        self.w = None
        self.r = {}


class KB:
    NDS = 56

    def __init__(self, nc, es):
        self.nc = nc
        self.E = {"pe": nc.tensor, "act": nc.scalar, "dve": nc.vector, "pool": nc.gpsimd, "sp": nc.sync}
        self.sem = {k: es.enter_context(nc.semaphore("sem_" + k)) for k in ("pe", "act", "dve")}
        self.cnt = {k: 0 for k in self.sem}
        self.dsem = [es.enter_context(nc.semaphore(f"dsem{i}")) for i in range(self.NDS)]
        self.dcnt = [0] * self.NDS
        self.drr = {"sp": 0, "pool": 0}
        self.tick = None
        self.pace = 28
        self.seen = {k: {} for k in self.E}
        self.nins = 0
        self.nwait = 0

    def _semof(self, key):
        return self.sem[key] if isinstance(key, str) else self.dsem[key]

    def _wait(self, eng, deps):
        best = {}
        for k, n in deps:
            if best.get(k, 0) < n:
                best[k] = n
        sn = self.seen[eng]
        for k, n in best.items():
            if sn.get(k, 0) >= n:
                continue
            self.E[eng].wait_ge(self._semof(k), n)
            self.nwait += 1
            sn[k] = n

    def _deps(self, eng, rd, wr):
        deps = []
        for r in rd:
            if r.w is not None and not (eng == "pe" and r.w[0] == "pe"):
                deps.append(r.w)
        for w in wr:
            if w.w is not None and w.w[0] != eng:
                deps.append(w.w)
            for k, n in w.r.items():
                if k != eng:
                    deps.append((k, n))
        return deps

    def _commit(self, tok, rd, wr):
        k, n = tok
        for r in rd:
            if r.r.get(k, 0) < n:
                r.r[k] = n
        for w in wr:
            w.w = tok
            w.r = {}

    def op(self, eng, fn, rd=(), wr=()):
        self._wait(eng, self._deps(eng, rd, wr))
        ins = fn()
        self.cnt[eng] += 1
        self.nins += 1
        ins.then_inc(self.sem[eng], 1)
        tok = (eng, self.cnt[eng])
        self._commit(tok, rd, wr)
        if eng == "pe" and self.tick is not None and self.cnt["pe"] % self.pace == 0:
            self.tick()
        return tok

    def dma(self, q, fn, rd=(), wr=()):
        half = self.NDS // 2
        i = (self.drr[q] % half) + (0 if q == "sp" else half)
        self.drr[q] += 1
        deps = self._deps(q, rd, wr)
        if self.dcnt[i] > 0:
            deps.append((i, self.dcnt[i]))
        self._wait(q, deps)
        ins = fn(self.E[q])
        self.dcnt[i] += 16
        self.nins += 1
        ins.then_inc(self.dsem[i], 16)
        tok = (i, self.dcnt[i])
        self._commit(tok, rd, wr)
        return tok

    def barrier(self, engs=("pe", "act", "dve", "pool", "sp")):
        deps = [(k, self.cnt[k]) for k in self.sem if self.cnt[k] > 0]
        deps += [(i, self.dcnt[i]) for i in range(self.NDS) if self.dcnt[i] > 0]
        for e in engs:
            self._wait(e, deps)


def _interleave(ga, gb, ratio):
    a_alive = ga is not None
    b_alive = gb is not None
    while a_alive or b_alive:
        if a_alive:
            for _ in range(ratio):
                try:
                    next(ga)
                except StopIteration:
                    a_alive = False
                    break
        if b_alive:
            try:
                next(gb)
            except StopIteration:
                b_alive = False


DBG = int(os.environ.get("KDBG", "0"))
NCONV = int(os.environ.get("KNCONV", "24"))


def build(stop="all", taps=()):
    nc = bass.Bass("TRN2", target_bir_lowering=False)

    def din(name, shape, dt=F32):
        return nc.dram_tensor(name, list(shape), dt, kind="ExternalInput").ap()

    def dscr(name, shape, dt):
        return nc.dram_tensor(name, list(shape), dt, kind="Internal").ap()

    x_d = din("x", [T, D])
    pos_d = din("pos", [1, T], I32)
    w_in_d = din("w_in", [D, IN_W])
    w_br_d = din("w_branch", [2, 1024, D])
    w_out_d = din("w_out", [D, D])
    wr_d = din("w_router", [D, 36])
    w1_d = din("w1", [NE, D, DE])
    w3_d = din("w3", [NE, D, DE])
    w2_d = din("w2", [NE, DE, D])
    nmw_d = din("nmwT", [128, KC])
    bf_d = din("bf_bc", [128, 128])
    gnw_d = din("gnwT", [128, NH])
    bg_d = din("bgT", [128, 32])
    nfw_d = din("nfw_bc", [128, D])
    nfin_d = din("nfin_bc", [128, D])
    rb_d = din("rbias_bc", [128, 36])
    cmat_d = din("cmat", [128, 6 * 128])
    cret_d = din("cret", [128, 2 * NH * 128 + NH + 2])
    ebase_d = din("ebase", [128, NT * NE])
    out_d = nc.dram_tensor("out", [T, D], F32, kind="ExternalOutput").ap()

    foxo_d = dscr("foxo_d", [NH, 128, T], BF16)
    reto_d = dscr("reto_d", [NH, 128, T], BF16)
    gates_d = dscr("gates_d", [2 * D, T], BF16)
    h_d = dscr("h_d", [T, D], F32)
    hn_d = dscr("hn_d", [T, D], BF16)
    xg_d = dscr("xg_d", [NSLOT + 128, D], BF16)
    y_d = dscr("y_d", [NSLOT + 128, D], BF16)

    wb1_d = dscr("wb1_d", [NE, 128, KC * DE], BF16)
    wb3_d = dscr("wb3_d", [NE, 128, KC * DE], BF16)
    wb2_d = dscr("wb2_d", [NE, 128, 4 * D], BF16)

    tap_out = {}

    es = contextlib.ExitStack()
    with es:
        kb = KB(nc, es)
        regs = {}

        def R(name):
            r = regs.get(name)
            if r is None:
                r = regs[name] = Reg()
            return r

        uniq = {"i": 0}

        def sbt(stack, name, shape, dt=F32):
            uniq["i"] += 1
            return stack.enter_context(nc.sbuf_tensor(f"s{uniq['i']}_{name}", list(shape), dt))

        ps = [es.enter_context(nc.psum_tensor(f"ps{i}", [128, 512], F32)) for i in range(8)]
        psb = [p[:].bitcast(BF16) for p in ps]
        Rps = [R(f"ps{i}") for i in range(8)]

        cmat_f = sbt(es, "cmat_f", [128, 6 * 128], F32)
        cmat_b = sbt(es, "cmat_b", [128, 6 * 128], BF16)
        kb.dma("sp", lambda q: q.dma_start(out=cmat_f[:], in_=cmat_d), wr=[R("cmat_f")])
        kb.dma("pool", lambda q: q.dma_start(out=cmat_b[:], in_=cmat_d), wr=[R("cmat_b")])
        identb = cmat_b[:, 0:128]
        maskTb = cmat_b[:, 128:256]
        Lstrb = cmat_b[:, 256:384]
        onesb = cmat_b[:, 384:512]
        Rswapb = cmat_b[:, 512:640]
        Ucumf = cmat_f[:, 128:256]
        onesf = cmat_f[:, 384:512]
        sel127f = cmat_f[:, 640:768]
        RCB = R("cmat_b")
        RCF = R("cmat_f")
        eps_t = sbt(es, "eps_t", [128, 1], F32)
        one_t = sbt(es, "one_t", [128, 1], F32)
        kb.op("dve", lambda: nc.vector.memset(eps_t[:], EPS), wr=[R("eps_t")])
        kb.op("dve", lambda: nc.vector.memset(one_t[:], 1.0), wr=[R("one_t")])

        NSTG = 6
        LAG = 2
        stg = [sbt(es, f"stg{i}", [128, 1024], BF16) for i in range(NSTG)]
        conv_units = []
        conv_set = [e for e in range(NE) if (e % 4 != 3 or NCONV >= NE)]
        for e in conv_set:
            for q8 in range(8):
                conv_units.append((e, "w1", q8))
                conv_units.append((e, "w3", q8))
                conv_units.append((e, "w2", q8))
        cstate = {"k": 0}

        def conv_in(k):
            e, mat, q8 = conv_units[k]
            sbuf = stg[k % NSTG]
            if mat == "w2":
                m, hf = q8 // 2, q8 % 2
                src = w2_d[e][m * 128:(m + 1) * 128, hf * 1024:(hf + 1) * 1024]
                dst = sbuf[:, :]
            else:
                wd = w1_d if mat == "w1" else w3_d
                src = wd[e][q8 * 256:(q8 + 1) * 256, :].rearrange("(kc p) n -> p kc n", p=128)
                dst = sbuf[:, :].rearrange("p (kc n) -> p kc n", n=DE)
            kb.dma("pool", lambda q: q.dma_start(out=dst, in_=src), wr=[R(f"stg{k % NSTG}")])

        def conv_out(k):
            e, mat, q8 = conv_units[k]
            sbuf = stg[k % NSTG]
            wd = {"w1": wb1_d, "w3": wb3_d, "w2": wb2_d}[mat]
            kb.dma("sp", lambda q: q.dma_start(out=wd[e][:, q8 * 1024:(q8 + 1) * 1024], in_=sbuf[:, :]),
                   rd=[R(f"stg{k % NSTG}")], wr=[R(f"wbu_{e}_{mat}_{q8}")])

        def conv_tick():
            k = cstate["k"]
            if k >= len(conv_units) + LAG:
                return
            if k >= LAG:
                conv_out(k - LAG)
            if k < len(conv_units):
                conv_in(k)
            cstate["k"] = k + 1

        def conv_flush():
            while cstate["k"] < len(conv_units) + LAG:
                conv_tick()

        NWR = 3
        pA = contextlib.ExitStack()
        es.enter_context(pA)
        wring = [sbt(pA, f"wring{i}", [128, KC, 256], BF16) for i in range(NWR)]
        Rwr = [R(f"wring{i}") for i in range(NWR)]
        wstate = {"n": 0}

        def load_w(c0, ncols=256):
            s = wstate["n"] % NWR
            wstate["n"] += 1
            src = w_in_d[:, c0:c0 + ncols].rearrange("(kc p) n -> p kc n", p=128)
            kb.dma("pool", lambda q: q.dma_start(out=wring[s][:, :, 0:ncols], in_=src), wr=[Rwr[s]])
            return s

        bank_rr = {"i": 0}

        def tap(name, sb_ap, shape, dt, rd):
            if name not in taps:
                return
            t = nc.dram_tensor("tap_" + name, list(shape), dt, kind="ExternalOutput").ap()
            tap_out[name] = t
            kb.dma("sp", lambda q: q.dma_start(out=t, in_=sb_ap), rd=rd, wr=[Reg()])

        def finish():
            kb.barrier(engs=("sp",))

        xnT = sbt(pA, "xnT", [128, KC, T], BF16)
        RxnT = [R(f"xnT{tc}") for tc in range(4)]
        nmwT = sbt(pA, "nmwT", [128, KC], F32)
        kb.dma("sp", lambda q: q.dma_start(out=nmwT[:], in_=nmw_d), wr=[R("nmwT")])

        with contextlib.ExitStack() as p0:
            xt = [sbt(p0, f"xt{i}", [128, D], F32) for i in range(3)]
            xs = [sbt(p0, f"xs{i}", [128, D], BF16) for i in range(3)]
            junk = sbt(p0, "junk0", [128, D], BF16)
            ss = sbt(p0, "ss0", [128, NT], F32)
            rstd = sbt(p0, "rstd0", [128, NT], F32)
            kb.op("dve", lambda: nc.vector.memset(ss[:], 0.0), wr=[R("ss0")])
            for i in range(NT):
                b = i % 3
                kb.dma("sp", lambda q: q.dma_start(out=xt[b][:], in_=x_d[i * 128:(i + 1) * 128, :]), wr=[R(f"xt{b}")])
                kb.op("act", lambda: nc.scalar.activation(out=junk[:], in_=xt[b][:], func=AF.Square,
                                                          accum_out=ss[:, i:i + 1]),
                      rd=[R(f"xt{b}"), R("ss0")], wr=[R("junk0"), R(f"ss0_{i}")])
                kb.op("act", lambda: nc.scalar.activation(out=rstd[:, i:i + 1], in_=ss[:, i:i + 1], func=AF.Sqrt,
                                                          bias=eps_t[:, 0:1], scale=1.0 / D),
                      rd=[R(f"ss0_{i}"), R("eps_t")], wr=[R(f"rstd0_{i}")])
                kb.op("dve", lambda: nc.vector.reciprocal(out=rstd[:, i:i + 1], in_=rstd[:, i:i + 1]),
                      rd=[R(f"rstd0_{i}")], wr=[R(f"rstd0_{i}")])
                def post0(i):
                    b = i % 3
                    kb.op("act", lambda: nc.scalar.mul(out=xs[b][:], in_=xt[b][:], mul=rstd[:, i:i + 1]),
                          rd=[R(f"xt{b}"), R(f"rstd0_{i}")], wr=[R(f"xs{b}")])
                    for half in range(2):
                        bk = (2 * i + half) % 4
                        for k in range(8):
                            kc = half * 8 + k
                            kb.op("pe", lambda: nc.tensor.transpose(out=psb[bk][:, k * 128:(k + 1) * 128],
                                                                    in_=xs[b][:, kc * 128:(kc + 1) * 128], identity=identb),
                                  rd=[R(f"xs{b}"), RCB], wr=[Rps[bk]])
                        kb.op("dve", lambda: nc.vector.tensor_tensor(
                            out=xnT[:, half * 8:(half + 1) * 8, i * 128:(i + 1) * 128],
                            in0=psb[bk][:, 0:1024].rearrange("p (k t) -> p k t", t=128),
                            in1=nmwT[:, half * 8:(half + 1) * 8].unsqueeze(2).to_broadcast([128, 8, 128]),
                            op=ALU.mult), rd=[Rps[bk], R("nmwT")], wr=[RxnT[i // 4]])
                if i >= 1:
                    post0(i - 1)
            post0(NT - 1)
            kb.barrier()
        tap("xnT", xnT[:, 0:2, :], [128, 2, T], BF16, RxnT)
        if NCONV > 0:
            kb.pace = max(4, int(12300 / (len(conv_units) + 1)))
            kb.tick = conv_tick
        if stop == "p0":
            finish()
            return nc, tap_out

        def proj_fm(slot, ncolchunks, evac, banks):
            for c in range(ncolchunks):
                for tc in range(4):
                    bk = banks[bank_rr["i"] % len(banks)]
                    bank_rr["i"] += 1
                    for kc in range(KC):
                        kb.op("pe", lambda: nc.tensor.matmul(ps[bk][:, :], lhsT=wring[slot][:, kc, c * 128:(c + 1) * 128],
                                                             rhs=xnT[:, kc, tc * 512:(tc + 1) * 512],
                                                             start=(kc == 0), stop=(kc == KC - 1)),
                              rd=[Rwr[slot], RxnT[tc]], wr=[Rps[bk]])
                    evac(c, tc, bk)
                    yield

        def proj_tm(slot, ncols, evac, banks):
            for i in range(NT):
                bk = banks[bank_rr["i"] % len(banks)]
                bank_rr["i"] += 1
                for kc in range(KC):
                    kb.op("pe", lambda: nc.tensor.matmul(ps[bk][:, 0:ncols], lhsT=xnT[:, kc, i * 128:(i + 1) * 128],
                                                         rhs=wring[slot][:, kc, 0:ncols],
                                                         start=(kc == 0), stop=(kc == KC - 1)),
                          rd=[Rwr[slot], RxnT[i // 4]], wr=[Rps[bk]])
                evac(i, bk)
                yield

        ev_rr = {"i": 0}

        def evac_copy(out_ap, in_ap, rd, wr, eng=None):
            e = "act" if ev_rr["i"] % 2 == 0 else "dve"
            if eng is not None:
                e = eng
            else:
                ev_rr["i"] += 1
            if e == "act":
                kb.op("act", lambda: nc.scalar.copy(out=out_ap, in_=in_ap), rd=rd, wr=wr)
            else:
                kb.op("dve", lambda: nc.vector.tensor_copy(out=out_ap, in_=in_ap), rd=rd, wr=wr)

        with contextlib.ExitStack() as p1:
            fqT = [sbt(p1, f"fqT{i}", [128, 2, T], BF16) for i in range(2)]
            fkT = [sbt(p1, f"fkT{i}", [128, 2, T], BF16) for i in range(2)]
            fvt = [sbt(p1, f"fvt{i}", [128, NT, 256], BF16) for i in range(2)]
            foT = [sbt(p1, f"foT{i}", [128, T], BF16) for i in range(2)]
            PT = [sbt(p1, f"PT{i}", [128, 512], BF16) for i in range(3)]
            rz = [sbt(p1, f"rz{i}", [128, 512], F32) for i in range(2)]
            biasT = sbt(p1, "biasT", [128, NH, NT, NT], F32)
            wf = sbt(p1, "wf", [128, KC, 8], BF16)
            bf_bc = sbt(p1, "bf_bc", [128, 128], F32)
            zt = sbt(p1, "zt", [128, 128], F32)
            spt = sbt(p1, "spt", [128, 128], F32)
            offs = sbt(p1, "offs", [128, NT, NH], F32)
            csp = sbt(p1, "csp", [128, NT, NH], F32)
            crefb = sbt(p1, "crefb", [128, NT, NH], F32)

            kb.dma("pool", lambda q: q.dma_start(
                out=wf[:], in_=w_in_d[:, C_FL:C_FL + 8].rearrange("(kc p) n -> p kc n", p=128)), wr=[R("wf")])
            kb.dma("sp", lambda q: q.dma_start(out=bf_bc[:], in_=bf_d), wr=[R("bf_bc")])
            order1 = []
            for g in range(4):
                order1 += [C_FQ + 256 * g, C_FK + 256 * g, C_FV + 256 * g]
            wq1 = {"next": 0, "slots": {}}

            def prefetch1():
                k = wq1["next"]
                if k < len(order1):
                    wq1["slots"][k] = load_w(order1[k])
                    wq1["next"] += 1

            for _ in range(NWR):
                prefetch1()

            for i in range(NT):
                for kc in range(KC):
                    kb.op("pe", lambda: nc.tensor.matmul(ps[0][:, i * 8:(i + 1) * 8], lhsT=xnT[:, kc, i * 128:(i + 1) * 128],
                                                         rhs=wf[:, kc, :], start=(kc == 0), stop=(kc == KC - 1)),
                          rd=[R("wf"), RxnT[i // 4]], wr=[Rps[0]])
            kb.op("dve", lambda: nc.vector.tensor_tensor(out=zt[:], in0=ps[0][:, 0:128], in1=bf_bc[:], op=ALU.add),
                  rd=[Rps[0], R("bf_bc")], wr=[R("zt")])
            kb.op("act", lambda: nc.scalar.activation(out=zt[:], in_=zt[:], func=AF.Exp, scale=-1.0),
                  rd=[R("zt")], wr=[R("zt")])
            kb.op("act", lambda: nc.scalar.activation(out=spt[:], in_=zt[:], func=AF.Ln, bias=one_t[:, 0:1], scale=1.0),
                  rd=[R("zt"), R("one_t")], wr=[R("spt")])
            tap("sp", spt[:], [128, 128], F32, [R("spt")])
            kb.op("pe", lambda: nc.tensor.matmul(ps[1][:, 0:128], lhsT=Ucumf, rhs=spt[:], start=True, stop=True),
                  rd=[RCF, R("spt")], wr=[Rps[1]])
            kb.op("pe", lambda: nc.tensor.matmul(ps[2][:, 0:128], lhsT=onesf, rhs=spt[:], start=True, stop=True),
                  rd=[RCF, R("spt")], wr=[Rps[2]])
            kb.op("dve", lambda: nc.vector.memset(offs[:], 0.0), wr=[R("offs")])
            for i in range(1, NT):
                kb.op("dve", lambda: nc.vector.tensor_tensor(out=offs[:, i, :], in0=offs[:, i - 1, :],
                                                             in1=ps[2][:, (i - 1) * 8:i * 8], op=ALU.add),
                      rd=[R("offs"), Rps[2]], wr=[R("offs")])
            kb.op("dve", lambda: nc.vector.tensor_tensor(out=csp[:].rearrange("p a b -> p (a b)"), in0=ps[1][:, 0:128],
                                                         in1=offs[:].rearrange("p a b -> p (a b)"), op=ALU.add),
                  rd=[R("offs"), Rps[1]], wr=[R("csp")])
            kb.op("pe", lambda: nc.tensor.matmul(ps[3][:, 0:128], lhsT=sel127f, rhs=csp[:].rearrange("p a b -> p (a b)"),
                                                 start=True, stop=True), rd=[RCF, R("csp")], wr=[Rps[3]])
            kb.op("dve", lambda: nc.vector.tensor_copy(out=crefb[:].rearrange("p a b -> p (a b)"), in_=ps[3][:, 0:128]),
                  rd=[Rps[3]], wr=[R("crefb")])
            for h in range(NH):
                kb.op("dve", lambda: nc.vector.tensor_tensor(
                    out=biasT[:, h, :, :], in0=csp[:, :, h].unsqueeze(2).to_broadcast([128, NT, NT]),
                    in1=crefb[:, :, h].unsqueeze(1).to_broadcast([128, NT, NT]), op=ALU.subtract),
                    rd=[R("csp"), R("crefb")], wr=[R("biasT")])
            tap("csp", csp[:], [128, NT, NH], F32, [R("csp")])

            def gen_proj1(g):
                b = g % 2
                for part, dst, nm in ((0, fqT, "fqT"), (1, fkT, "fkT")):
                    slot = wq1["slots"][3 * g + part]

                    def ev(c, tc, bk, dst=dst, nm=nm):
                        evac_copy(dst[b][:, c, tc * 512:(tc + 1) * 512], ps[bk][:, :], [Rps[bk]], [R(f"{nm}{b}_{tc}")])
                    yield from proj_fm(slot, 2, ev, (6, 7))
                    prefetch1()
                slot = wq1["slots"][3 * g + 2]

                def evv(i, bk):
                    evac_copy(fvt[b][:, i, :], ps[bk][:, 0:256], [Rps[bk]], [R(f"fvt{b}_{i // 4}")])
                yield from proj_tm(slot, 256, evv, (6, 7))
                prefetch1()

            scale = 1.0 / math.sqrt(HD)
            pt_rr = {"i": 0}

            deferred1 = []

            def flush_deferred1():
                while deferred1:
                    deferred1.pop(0)()

            def gen_attn(g):
                b = g % 2
                for hh in range(2):
                    h = 2 * g + hh
                    fb = h % 2
                    for qc in range(4):
                        if qc == 1:
                            flush_deferred1()
                        ob = 2 + (qc % 2)
                        zb = 4 + (qc % 2)
                        nkb = 4 * qc + 4
                        pend = None
                        for kbk in range(nkb + 1):
                            if kbk < nkb:
                                j0 = max(0, kbk - 4 * qc)
                                sbk = kbk % 2
                                c0 = j0 * 128
                                kb.op("pe", lambda: nc.tensor.matmul(
                                    ps[sbk][:, c0:512], lhsT=fkT[b][:, hh, kbk * 128:(kbk + 1) * 128],
                                    rhs=fqT[b][:, hh, qc * 512 + c0:(qc + 1) * 512], start=True, stop=True),
                                    rd=[R(f"fkT{b}_{kbk // 4}"), R(f"fqT{b}_{qc}")], wr=[Rps[sbk]])
                                pi = pt_rr["i"] % 3
                                pt_rr["i"] += 1
                                for j in range(j0, 4):
                                    qb = 4 * qc + j
                                    kb.op("act", lambda: nc.scalar.activation(
                                        out=PT[pi][:, j * 128:(j + 1) * 128], in_=ps[sbk][:, j * 128:(j + 1) * 128],
                                        func=AF.Exp, bias=biasT[:, h, kbk, qb:qb + 1], scale=scale),
                                        rd=[Rps[sbk], R("biasT")], wr=[R(f"PT{pi}")])
                                if kbk >= 4 * qc:
                                    kb.op("dve", lambda: nc.vector.tensor_tensor(
                                        out=PT[pi][:, c0:c0 + 128], in0=PT[pi][:, c0:c0 + 128], in1=maskTb, op=ALU.mult),
                                        rd=[R(f"PT{pi}"), RCB], wr=[R(f"PT{pi}")])
                                cur = (kbk, pi, c0)
                            else:
                                cur = None
                            if pend is not None:
                                pk, ppi, pc0 = pend
                                kb.op("pe", lambda: nc.tensor.matmul(
                                    ps[ob][:, pc0:512], lhsT=fvt[b][:, pk, hh * 128:(hh + 1) * 128], rhs=PT[ppi][:, pc0:512],
                                    start=(pk == 0), stop=(pk == nkb - 1)),
                                    rd=[R(f"fvt{b}_{pk // 4}"), R(f"PT{ppi}")], wr=[Rps[ob]])
                                kb.op("pe", lambda: nc.tensor.matmul(
                                    ps[zb][:, pc0:512], lhsT=onesb, rhs=PT[ppi][:, pc0:512],
                                    start=(pk == 0), stop=(pk == nkb - 1)),
                                    rd=[RCB, R(f"PT{ppi}")], wr=[Rps[zb]])
                            pend = cur
                            yield
                        rzi = qc % 2
                        kb.op("dve", lambda: nc.vector.reciprocal(out=rz[rzi][:], in_=ps[zb][:, :]),
                              rd=[Rps[zb]], wr=[R(f"rz{rzi}")])
                        kb.op("dve", lambda: nc.vector.tensor_tensor(out=foT[fb][:, qc * 512:(qc + 1) * 512],
                                                                     in0=ps[ob][:, :], in1=rz[rzi][:], op=ALU.mult),
                              rd=[Rps[ob], R(f"rz{rzi}")], wr=[R(f"foT{fb}")])
                    deferred1.append(lambda h=h, fb=fb: kb.dma(
                        "sp", lambda q: q.dma_start(out=foxo_d[h], in_=foT[fb][:]), rd=[R(f"foT{fb}")], wr=[R(f"foxo_d{h}")]))
                    if h == 0:
                        tap("foT0", foT[fb][:], [128, T], BF16, [R(f"foT{fb}")])

            for _ in gen_proj1(0):
                pass
            tap("fqT0", fqT[0][:, 0, :], [128, T], BF16, [R(f"fqT0_{tc}") for tc in range(4)])
            tap("fvt0", fvt[0][:], [128, NT, 256], BF16, [R(f"fvt0_{tc}") for tc in range(4)])
            for g in range(4):
                _interleave(gen_attn(g), gen_proj1(g + 1) if g < 3 else None, 3)
            flush_deferred1()
            kb.barrier()
        if stop == "p1":
            finish()
            return nc, tap_out

        NC2 = 2 * NH * 128 + NH + 2
        with contextlib.ExitStack() as p2:
            cret = sbt(p2, "cret", [128, NC2], F32)
            kb.dma("sp", lambda q: q.dma_start(out=cret[:], in_=cret_d), wr=[R("cret")])
            RCR = R("cret")
            decT = cret[:, 0:1024]
            xib = cret[:, 1024:2048]
            zeta = cret[:, 2048:2056]
            invf = cret[:, 2056:2057]
            ssign = cret[:, 2057:2058]
            gnwT = sbt(p2, "gnwT", [128, NH], F32)
            kb.dma("sp", lambda q: q.dma_start(out=gnwT[:], in_=gnw_d), wr=[R("gnwT")])
            cosT = sbt(p2, "cosT", [128, T], F32)
            sinS = sbt(p2, "sinS", [128, T], F32)
            order2 = []
            for g in range(NH):
                order2 += [C_RQ + 128 * g, C_RK + 128 * g, C_RV + 128 * g, C_RG + 128 * g]
            wq2 = {"next": 0, "slots": {}}

            def prefetch2():
                k = wq2["next"]
                if k < len(order2):
                    wq2["slots"][k] = load_w(order2[k], 128)
                    wq2["next"] += 1

            for _ in range(NWR):
                prefetch2()
            with contextlib.ExitStack() as p2a:
                posi = sbt(p2a, "posi", [128, T], I32)
                ang = sbt(p2a, "ang", [128, T], F32)
                tq = sbt(p2a, "tq", [128, T], F32)
                ki = sbt(p2a, "ki", [128, T], I32)
                kb.dma("sp", lambda q: q.dma_start(out=posi[:], in_=pos_d.partition_broadcast(128)), wr=[R("posi")])
                kb.op("dve", lambda: nc.vector.tensor_copy(out=ang[:], in_=posi[:]), rd=[R("posi")], wr=[R("ang")])
                kb.op("dve", lambda: nc.vector.tensor_scalar(out=ang[:], in0=ang[:], scalar1=invf, scalar2=None, op0=ALU.mult),
                      rd=[R("ang"), RCR], wr=[R("ang")])
                C1 = 6.28125
                C2 = 2.0 * PI - C1
                for which, dst, shift in (("sin", sinS, 0.0), ("cos", cosT, PI / 2)):
                    kb.op("dve", lambda: nc.vector.tensor_scalar(out=tq[:], in0=ang[:], scalar1=shift, scalar2=1.0 / (2 * PI),
                                                                 op0=ALU.add, op1=ALU.mult), rd=[R("ang")], wr=[R("tq")])
                    kb.op("dve", lambda: nc.vector.tensor_copy(out=ki[:], in_=tq[:]), rd=[R("tq")], wr=[R("ki")])
                    kb.op("dve", lambda: nc.vector.tensor_copy(out=tq[:], in_=ki[:]), rd=[R("ki")], wr=[R("tq")])
                    kb.op("dve", lambda: nc.vector.scalar_tensor_tensor(out=dst[:], in0=tq[:], scalar=-C1, in1=ang[:],
                                                                        op0=ALU.mult, op1=ALU.add),
                          rd=[R("tq"), R("ang")], wr=[R(which)])
                    kb.op("dve", lambda: nc.vector.scalar_tensor_tensor(out=dst[:], in0=tq[:], scalar=-C2, in1=dst[:],
                                                                        op0=ALU.mult, op1=ALU.add),
                          rd=[R("tq"), R(which)], wr=[R(which)])
                    kb.op("dve", lambda: nc.vector.tensor_scalar(out=dst[:], in0=dst[:], scalar1=shift, scalar2=-PI,
                                                                 op0=ALU.add, op1=ALU.max), rd=[R(which)], wr=[R(which)])
                    kb.op("dve", lambda: nc.vector.tensor_scalar(out=dst[:], in0=dst[:], scalar1=PI, scalar2=None,
                                                                 op0=ALU.min), rd=[R(which)], wr=[R(which)])
                    kb.op("act", lambda: nc.scalar.activation(out=dst[:], in_=dst[:], func=AF.Sin), rd=[R(which)], wr=[R(which)])
                kb.op("dve", lambda: nc.vector.tensor_scalar(out=sinS[:], in0=sinS[:], scalar1=ssign, scalar2=None, op0=ALU.mult),
                      rd=[R("sin"), RCR], wr=[R("sin")])
                kb.barrier()
            tap("cosT", cosT[:], [128, T], F32, [R("cos")])
            tap("sinS", sinS[:], [128, T], F32, [R("sin")])
            if stop == "p2a":
                kb.barrier()
                finish()
                return nc, tap_out

            rqraw = [sbt(p2, f"rqraw{i}", [128, 1, T], BF16) for i in range(2)]
            rkraw = [sbt(p2, f"rkraw{i}", [128, 1, T], BF16) for i in range(2)]
            rvt = [sbt(p2, f"rvt{i}", [128, NT, 128], BF16) for i in range(2)]
            srg = [sbt(p2, f"srg{i}", [128, 1, T], BF16) for i in range(2)]
            qrot = sbt(p2, "qrot", [128, T], BF16)
            krot = sbt(p2, "krot", [128, T], BF16)
            qxi = sbt(p2, "qxi", [128, T], BF16)
            kz = sbt(p2, "kz", [128, NT, 128], BF16)
            stb = sbt(p2, "stb", [128, NT, 128], BF16)
            st = sbt(p2, "st", [128, 128], F32)
            tA = [sbt(p2, f"tA{i}", [128, 512], F32) for i in range(2)]
            tB = [sbt(p2, f"tB{i}", [128, 512], F32) for i in range(2)]
            innb = [sbt(p2, f"innb{i}", [128, 4, 128], BF16) for i in range(2)]
            ynb = [sbt(p2, f"ynb{i}", [128, 4, 128], BF16) for i in range(2)]
            sq = sbt(p2, "sq", [128, 512], F32)
            stat = sbt(p2, "stat", [128, 6, 4], F32)
            roT = [sbt(p2, f"roT{i}", [128, T], BF16) for i in range(2)]

            def gen_proj2(g):
                b = g % 2
                for part, dst, nm in ((0, rqraw, "rqraw"), (1, rkraw, "rkraw")):
                    slot = wq2["slots"][4 * g + part]

                    def ev(c, tc, bk, dst=dst, nm=nm):
                        evac_copy(dst[b][:, c, tc * 512:(tc + 1) * 512], ps[bk][:, :], [Rps[bk]], [R(f"{nm}{b}_{tc}")])
                    yield from proj_fm(slot, 1, ev, (6, 7))
                    prefetch2()
                slot = wq2["slots"][4 * g + 2]

                def evv(i, bk):
                    evac_copy(rvt[b][:, i, :], ps[bk][:, 0:128], [Rps[bk]], [R(f"rvt{b}_{i // 4}")])
                yield from proj_tm(slot, 128, evv, (6, 7))
                prefetch2()
                slot = wq2["slots"][4 * g + 3]

                def evg(c, tc, bk):
                    kb.op("act", lambda: nc.scalar.activation(out=srg[b][:, c, tc * 512:(tc + 1) * 512], in_=ps[bk][:, :],
                                                              func=AF.Silu), rd=[Rps[bk]], wr=[R(f"srg{b}_{tc}")])
                yield from proj_fm(slot, 1, evg, (6, 7))
                prefetch2()

            tmp_rr = {"i": 0}

            deferred2 = []

            def flush_deferred2():
                while deferred2:
                    deferred2.pop(0)()

            def gen_ret(g):
                b = g % 2
                for hh in range(1):
                    h = g
                    rb = h % 2
                    gch = float(np.exp(np.float32(128.0) * np.log1p(-np.float32(2.0 ** (-5.0 - h)))))
                    for tc in range(4):
                        sl = slice(tc * 512, (tc + 1) * 512)
                        for src, dst, nm, bk in ((rqraw, qrot, "qrot", 0), (rkraw, krot, "krot", 1)):
                            ti = tmp_rr["i"] % 2
                            tmp_rr["i"] += 1
                            kb.op("pe", lambda: nc.tensor.matmul(ps[bk][:, :], lhsT=Rswapb, rhs=src[b][:, hh, sl],
                                                                 start=True, stop=True),
                                  rd=[RCB, R(f"r{nm[0]}raw{b}_{tc}")], wr=[Rps[bk]])
                            kb.op("dve", lambda: nc.vector.tensor_tensor(out=tA[ti][:], in0=src[b][:, hh, sl], in1=cosT[:, sl],
                                                                         op=ALU.mult),
                                  rd=[R(f"r{nm[0]}raw{b}_{tc}"), R("cos")], wr=[R(f"tA{ti}")])
                            kb.op("dve", lambda: nc.vector.tensor_tensor(out=tB[ti][:], in0=ps[bk][:, :], in1=sinS[:, sl],
                                                                         op=ALU.mult),
                                  rd=[Rps[bk], R("sin")], wr=[R(f"tB{ti}")])
                            kb.op("dve", lambda: nc.vector.tensor_tensor(out=dst[:, sl], in0=tA[ti][:], in1=tB[ti][:], op=ALU.add),
                                  rd=[R(f"tA{ti}"), R(f"tB{ti}")], wr=[R(f"{nm}_{tc}")])
                        kb.op("dve", lambda: nc.vector.tensor_tensor(
                            out=qxi[:, sl].rearrange("p (c n) -> p c n", n=128),
                            in0=qrot[:, sl].rearrange("p (c n) -> p c n", n=128),
                            in1=xib[:, h * 128:(h + 1) * 128].unsqueeze(1).to_broadcast([128, 4, 128]), op=ALU.mult),
                            rd=[R(f"qrot_{tc}"), RCR], wr=[R(f"qxi_{tc}")])
                        yield
                    if h == 0:
                        tap("qrot0", qrot[:], [128, T], BF16, [R(f"qrot_{tc}") for tc in range(4)])
                        tap("krot0", krot[:], [128, T], BF16, [R(f"krot_{tc}") for tc in range(4)])
                    for half in range(2):
                        for k in range(8):
                            c = half * 8 + k
                            kb.op("pe", lambda: nc.tensor.transpose(out=psb[2][:, k * 128:(k + 1) * 128],
                                                                    in_=krot[:, c * 128:(c + 1) * 128], identity=identb),
                                  rd=[R(f"krot_{c // 4}"), RCB], wr=[Rps[2]])
                        kb.op("dve", lambda: nc.vector.tensor_scalar(
                            out=kz[:, half * 8:(half + 1) * 8, :], in0=psb[2][:, 0:1024].rearrange("p (k t) -> p k t", t=128),
                            scalar1=zeta[:, h:h + 1], scalar2=None, op0=ALU.mult), rd=[Rps[2], RCR], wr=[R("kz")])
                        yield
                    flush_deferred2()
                    kb.op("dve", lambda: nc.vector.memset(st[:], 0.0), wr=[R("st")])
                    for c4 in range(4):
                        for cc in range(4):
                            c = 4 * c4 + cc
                            if c == 15:
                                continue
                            kb.op("pe", lambda: nc.tensor.matmul(ps[3][:, cc * 128:(cc + 1) * 128], lhsT=kz[:, c, :],
                                                                 rhs=rvt[b][:, c, hh * 128:(hh + 1) * 128], start=True, stop=True),
                                  rd=[R("kz"), R(f"rvt{b}_{c // 4}")], wr=[Rps[3]])
                        for cc in range(4):
                            c = 4 * c4 + cc
                            if c == 15:
                                continue
                            kb.op("dve", lambda: nc.vector.scalar_tensor_tensor(
                                out=st[:], in0=st[:], scalar=gch, in1=ps[3][:, cc * 128:(cc + 1) * 128],
                                op0=ALU.mult, op1=ALU.add), rd=[R("st"), Rps[3]], wr=[R("st")])
                            kb.op("act", lambda: nc.scalar.copy(out=stb[:, c + 1, :], in_=st[:]), rd=[R("st")], wr=[R("stb")])
                        yield
                    for c4 in range(4):
                        ib = c4 % 2
                        sl = slice(c4 * 512, (c4 + 1) * 512)
                        for cc in range(4):
                            c = 4 * c4 + cc
                            cs = slice(c * 128, (c + 1) * 128)
                            kb.op("pe", lambda: nc.tensor.matmul(ps[4][:, cc * 128:(cc + 1) * 128], lhsT=krot[:, cs], rhs=qrot[:, cs],
                                                                 start=True, stop=True),
                                  rd=[R(f"krot_{c4}"), R(f"qrot_{c4}")], wr=[Rps[4]])
                        kb.op("dve", lambda: nc.vector.tensor_tensor(
                            out=innb[ib][:], in0=ps[4][:, :].rearrange("p (c n) -> p c n", n=128),
                            in1=decT[:, h * 128:(h + 1) * 128].unsqueeze(1).to_broadcast([128, 4, 128]), op=ALU.mult),
                            rd=[Rps[4], RCR], wr=[R(f"innb{ib}")])
                        if DBG == 1:
                            yield
                            continue
                        for cc in range(4):
                            c = 4 * c4 + cc
                            cs = slice(c * 128, (c + 1) * 128)
                            kb.op("pe", lambda: nc.tensor.matmul(ps[5][:, cc * 128:(cc + 1) * 128], lhsT=innb[ib][:, cc, :],
                                                                 rhs=rvt[b][:, c, hh * 128:(hh + 1) * 128],
                                                                 start=True, stop=(c == 0)),
                                  rd=[R(f"innb{ib}"), R(f"rvt{b}_{c4}")], wr=[Rps[5]])
                            if c > 0:
                                kb.op("pe", lambda: nc.tensor.matmul(ps[5][:, cc * 128:(cc + 1) * 128], lhsT=qxi[:, cs],
                                                                     rhs=stb[:, c, :], start=False, stop=True),
                                      rd=[R(f"qxi_{c4}"), R("stb")], wr=[Rps[5]])
                        if h == 0 and c4 == 0:
                            kb.op("dve", lambda: nc.vector.tensor_copy(out=tA[0][:], in_=ps[5][:, :]), rd=[Rps[5]], wr=[R("tA0")])
                            tap("retraw0", tA[0][:], [128, 512], F32, [R("tA0")])
                        if DBG == 2:
                            yield
                            continue
                        kb.op("dve", lambda: nc.vector.tensor_copy(out=tA[1][:], in_=ps[5][:, :]), rd=[Rps[5]], wr=[R("tA1")])
                        o3 = tA[1][:].rearrange("p (c n) -> p c n", n=128)
                        kb.op("dve", lambda: nc.vector.tensor_reduce(out=stat[:, 0, :], in_=o3, axis=AX.X, op=ALU.add),
                              rd=[R("tA1")], wr=[R("stat0")])
                        if DBG == 31:
                            yield
                            continue
                        kb.op("act", lambda: nc.scalar.activation(out=sq[:], in_=tA[1][:], func=AF.Square),
                              rd=[R("tA1")], wr=[R("sq")])
                        if DBG == 32:
                            yield
                            continue
                        kb.op("dve", lambda: nc.vector.tensor_reduce(out=stat[:, 1, :], in_=sq[:].rearrange("p (c n) -> p c n", n=128),
                                                                     axis=AX.X, op=ALU.add), rd=[R("sq")], wr=[R("stat1")])
                        if DBG == 3:
                            yield
                            continue
                        kb.op("dve", lambda: nc.vector.tensor_scalar(out=stat[:, 2, :], in0=stat[:, 0, :], scalar1=1.0 / 128,
                                                                     scalar2=None, op0=ALU.mult), rd=[R("stat0")], wr=[R("stat2")])
                        kb.op("dve", lambda: nc.vector.tensor_tensor(out=stat[:, 3, :], in0=stat[:, 2, :], in1=stat[:, 2, :],
                                                                     op=ALU.mult), rd=[R("stat2")], wr=[R("stat3")])
                        kb.op("dve", lambda: nc.vector.scalar_tensor_tensor(out=stat[:, 4, :], in0=stat[:, 1, :], scalar=1.0 / 128,
                                                                            in1=stat[:, 3, :], op0=ALU.mult, op1=ALU.subtract),
                              rd=[R("stat1"), R("stat3")], wr=[R("stat4")])
                        kb.op("act", lambda: nc.scalar.activation(out=stat[:, 5, :], in_=stat[:, 4, :], func=AF.Sqrt,
                                                                  bias=eps_t[:, 0:1], scale=1.0),
                              rd=[R("stat4"), R("eps_t")], wr=[R("stat5")])
                        kb.op("dve", lambda: nc.vector.reciprocal(out=stat[:, 5, :], in_=stat[:, 5, :]),
                              rd=[R("stat5")], wr=[R("stat5")])
                        if DBG == 4:
                            yield
                            continue
                        kb.op("dve", lambda: nc.vector.tensor_tensor(
                            out=tA[1][:].rearrange("p (c n) -> p c n", n=128), in0=o3,
                            in1=stat[:, 2, :].unsqueeze(2).to_broadcast([128, 4, 128]), op=ALU.subtract),
                            rd=[R("tA1"), R("stat2")], wr=[R("tA1")])
                        kb.op("dve", lambda: nc.vector.tensor_tensor(
                            out=ynb[ib][:], in0=tA[1][:].rearrange("p (c n) -> p c n", n=128),
                            in1=stat[:, 5, :].unsqueeze(2).to_broadcast([128, 4, 128]), op=ALU.mult),
                            rd=[R("tA1"), R("stat5")], wr=[R(f"ynb{ib}")])
                        if DBG == 5:
                            yield
                            continue
                        for cc in range(4):
                            kb.op("pe", lambda: nc.tensor.transpose(out=psb[2][:, cc * 128:(cc + 1) * 128], in_=ynb[ib][:, cc, :],
                                                                    identity=identb),
                                  rd=[R(f"ynb{ib}"), RCB], wr=[Rps[2]])
                        kb.op("dve", lambda: nc.vector.scalar_tensor_tensor(
                            out=roT[rb][:, sl], in0=psb[2][:, 0:512], scalar=gnwT[:, h:h + 1], in1=srg[b][:, hh, sl],
                            op0=ALU.mult, op1=ALU.mult), rd=[Rps[2], R("gnwT"), R(f"srg{b}_{c4}")], wr=[R(f"roT{rb}")])
                        yield
                    deferred2.append(lambda h=h, rb=rb: kb.dma(
                        "sp", lambda q: q.dma_start(out=reto_d[h], in_=roT[rb][:]), rd=[R(f"roT{rb}")], wr=[R(f"reto_d{h}")]))
                    if h == 0:
                        tap("roT0", roT[rb][:], [128, T], BF16, [R(f"roT{rb}")])

            for _ in gen_proj2(0):
                pass
            if stop == "p2b":
                kb.barrier()
                finish()
                return nc, tap_out
            if stop.startswith("p2c"):
                nun = int(stop.split(":")[1]) if ":" in stop else 1000
                for iu, _ in enumerate(gen_ret(0)):
                    if iu + 1 >= nun:
                        break
                kb.barrier()
                finish()
                return nc, tap_out
            for g in range(NH):
                if g < NH - 1:
                    _interleave(gen_proj2(g + 1), gen_ret(g), 2)
                else:
                    _interleave(gen_ret(g), None, 1)
            flush_deferred2()
            kb.barrier()
        if stop == "p2":
            finish()
            return nc, tap_out

        with contextlib.ExitStack() as p3:
            bgT = sbt(p3, "bgT", [128, 32], F32)
            kb.dma("sp", lambda q: q.dma_start(out=bgT[:], in_=bg_d), wr=[R("bgT")])
            gst = [sbt(p3, f"gst{i}", [128, 512], BF16) for i in range(4)]
            slots3 = {}
            nxt = {"k": 0}

            def prefetch3():
                k = nxt["k"]
                if k < 16:
                    slots3[k] = load_w(C_GATE + 256 * k)
                    nxt["k"] += 1
            for _ in range(NWR):
                prefetch3()
            gs_rr = {"i": 0}
            for k in range(16):
                def evs(c, tc, bk, k=k):
                    colchunk = 2 * k + c
                    gi = gs_rr["i"] % 4
                    gs_rr["i"] += 1
                    kb.op("act", lambda: nc.scalar.activation(out=gst[gi][:], in_=ps[bk][:, :], func=AF.Sigmoid,
                                                              bias=bgT[:, colchunk:colchunk + 1], scale=1.0),
                          rd=[Rps[bk], R("bgT")], wr=[R(f"gst{gi}")])
                    kb.dma("sp", lambda q: q.dma_start(out=gates_d[colchunk * 128:(colchunk + 1) * 128, tc * 512:(tc + 1) * 512],
                                                       in_=gst[gi][:]), rd=[R(f"gst{gi}")], wr=[R(f"gates_d{colchunk}_{tc}")])
                for _ in proj_fm(slots3[k], 2, evs, (0, 1, 2, 3, 4, 5, 6, 7)):
                    pass
                prefetch3()
            kb.barrier()
        pA.close()
        if stop == "p3":
            finish()
            return nc, tap_out

        pB = contextlib.ExitStack()
        es.enter_context(pB)
        logits = sbt(pB, "logits", [128, NT, 36], F32)
        p4 = contextlib.ExitStack()
        es.enter_context(p4)
        mergedT = sbt(p4, "mergedT", [128, KC, T], BF16)
        with contextlib.ExitStack() as p4a:
            foA = sbt(p4a, "foA", [128, NH, T], BF16)
            roA = sbt(p4a, "roA", [128, NH, T], BF16)
            for h in range(NH):
                kb.dma("sp", lambda q: q.dma_start(out=foA[:, h, :], in_=foxo_d[h]), rd=[R(f"foxo_d{h}")], wr=[R(f"foA{h}")])
                kb.dma("sp", lambda q: q.dma_start(out=roA[:, h, :], in_=reto_d[h]), rd=[R(f"reto_d{h}")], wr=[R(f"roA{h}")])
            wbr = [sbt(p4a, f"wbr{i}", [128, 2, NH, 256], BF16) for i in range(2)]
            gt = [sbt(p4a, f"gt{i}", [128, 2, 512], BF16) for i in range(3)]
            m0 = [sbt(p4a, f"m0_{i}", [128, 512], F32) for i in range(2)]
            m1 = [sbt(p4a, f"m1_{i}", [128, 512], F32) for i in range(2)]

            def load_wbr(jp):
                s = jp % 2
                for n in range(2):
                    src = w_br_d[n][:, jp * 256:(jp + 1) * 256].rearrange("(h p) n -> p h n", p=128)
                    kb.dma("pool", lambda q: q.dma_start(out=wbr[s][:, n, :, :], in_=src), wr=[R(f"wbr{s}_{n}")])
            load_wbr(0)
            load_wbr(1)
            it = 0
            for jp in range(8):
                s = jp % 2
                for jj in range(2):
                    j = 2 * jp + jj
                    for tc in range(4):
                        gi = it % 3
                        mi = it % 2
                        it += 1
                        for n in range(2):
                            row0 = n * D + j * 128
                            kb.dma("sp", lambda q: q.dma_start(out=gt[gi][:, n, :],
                                                               in_=gates_d[row0:row0 + 128, tc * 512:(tc + 1) * 512]),
                                   rd=[R(f"gates_d{n * 16 + j}_{tc}")], wr=[R(f"gt{gi}_{n}")])
                        b0 = (2 * it) % 8
                        b1 = (2 * it + 1) % 8
                        for n, bk, src, rn in ((0, b0, foA, "foA"), (1, b1, roA, "roA")):
                            for h in range(NH):
                                kb.op("pe", lambda: nc.tensor.matmul(ps[bk][:, :], lhsT=wbr[s][:, n, h, jj * 128:(jj + 1) * 128],
                                                                     rhs=src[:, h, tc * 512:(tc + 1) * 512],
                                                                     start=(h == 0), stop=(h == NH - 1)),
                                      rd=[R(f"wbr{s}_{n}"), R(f"{rn}{h}")], wr=[Rps[bk]])
                        kb.op("dve", lambda: nc.vector.tensor_tensor(out=m0[mi][:], in0=ps[b0][:, :], in1=gt[gi][:, 0, :], op=ALU.mult),
                              rd=[Rps[b0], R(f"gt{gi}_0")], wr=[R(f"m0_{mi}")])
                        kb.op("dve", lambda: nc.vector.tensor_tensor(out=m1[mi][:], in0=ps[b1][:, :], in1=gt[gi][:, 1, :], op=ALU.mult),
                              rd=[Rps[b1], R(f"gt{gi}_1")], wr=[R(f"m1_{mi}")])
                        kb.op("dve", lambda: nc.vector.tensor_tensor(out=mergedT[:, j, tc * 512:(tc + 1) * 512], in0=m0[mi][:],
                                                                     in1=m1[mi][:], op=ALU.add),
                              rd=[R(f"m0_{mi}"), R(f"m1_{mi}")], wr=[R(f"mergedT{tc}")])
                if jp + 2 < 8:
                    load_wbr(jp + 2)
            kb.barrier()
        tap("mergedT", mergedT[:, 0:2, :], [128, 2, T], BF16, [R(f"mergedT{tc}") for tc in range(4)])
        if stop == "p4a":
            finish()
            return nc, tap_out

        with contextlib.ExitStack() as p4b:
            wout = sbt(p4b, "wout", [128, KC, D], BF16)
            for cg in range(4):
                kb.dma("pool", lambda q: q.dma_start(
                    out=wout[:, :, cg * 512:(cg + 1) * 512],
                    in_=w_out_d[:, cg * 512:(cg + 1) * 512].rearrange("(kc p) n -> p kc n", p=128)), wr=[R(f"wout{cg}")])
            wr_s = sbt(p4b, "wr_s", [128, KC, 36], BF16)
            kb.dma("pool", lambda q: q.dma_start(out=wr_s[:], in_=wr_d.rearrange("(kc p) n -> p kc n", p=128)), wr=[R("wr_s")])
            nfw = sbt(p4b, "nfw", [128, D], F32)
            kb.dma("sp", lambda q: q.dma_start(out=nfw[:], in_=nfw_d), wr=[R("nfw")])
            rbias = sbt(p4b, "rbias", [128, 36], F32)
            kb.dma("sp", lambda q: q.dma_start(out=rbias[:], in_=rb_d), wr=[R("rbias")])
            xt = [sbt(p4b, f"xt4_{i}", [128, D], F32) for i in range(2)]
            ht = [sbt(p4b, f"ht{i}", [128, D], F32) for i in range(2)]
            hnb = [sbt(p4b, f"hnb{i}", [128, D], BF16) for i in range(2)]
            hnT = [sbt(p4b, "hnT0", [128, KC, 128], BF16)] * 2
            junk = sbt(p4b, "junk4", [128, D], BF16)
            ss = sbt(p4b, "ss4", [128, NT], F32)
            rstd = sbt(p4b, "rstd4", [128, NT], F32)
            kb.op("dve", lambda: nc.vector.memset(ss[:], 0.0), wr=[R("ss4")])
            def post4a(i):
                b = i % 2
                for half in range(2):
                    bk = 4 + half
                    for k in range(8):
                        kc = half * 8 + k
                        kb.op("pe", lambda: nc.tensor.transpose(out=psb[bk][:, k * 128:(k + 1) * 128],
                                                                in_=hnb[b][:, kc * 128:(kc + 1) * 128], identity=identb),
                              rd=[R(f"hnb{b}"), RCB], wr=[Rps[bk]])
                    evac_copy(hnT[b][:, half * 8:(half + 1) * 8, :], psb[bk][:, 0:1024].rearrange("p (k t) -> p k t", t=128),
                              [Rps[bk]], [R("hnT")], eng="dve")

            def post4b(i):
                b = i % 2
                for kc in range(KC):
                    kb.op("pe", lambda: nc.tensor.matmul(ps[6][:, 0:36], lhsT=hnT[b][:, kc, :], rhs=wr_s[:, kc, :],
                                                         start=(kc == 0), stop=(kc == KC - 1)),
                          rd=[R("hnT"), R("wr_s")], wr=[Rps[6]])
                kb.op("dve", lambda: nc.vector.tensor_tensor(out=logits[:, i, :], in0=ps[6][:, 0:36], in1=rbias[:], op=ALU.add),
                      rd=[Rps[6], R("rbias")], wr=[R("logits")])

            def mm4(i, cg):
                b = i % 2
                for kc in range(KC):
                    kb.op("pe", lambda: nc.tensor.matmul(ps[cg][:, :], lhsT=mergedT[:, kc, i * 128:(i + 1) * 128],
                                                         rhs=wout[:, kc, cg * 512:(cg + 1) * 512],
                                                         start=(kc == 0), stop=(kc == KC - 1)),
                          rd=[R(f"mergedT{i // 4}"), R(f"wout{cg}")], wr=[Rps[cg]])
                kb.op("dve", lambda: nc.vector.tensor_tensor(out=ht[b][:, cg * 512:(cg + 1) * 512], in0=ps[cg][:, :],
                                                             in1=xt[b][:, cg * 512:(cg + 1) * 512], op=ALU.add),
                      rd=[Rps[cg], R(f"xt4_{b}")], wr=[R(f"ht{b}")])

            kb.dma("sp", lambda q: q.dma_start(out=xt[0][:], in_=x_d[0:128, :]), wr=[R("xt4_0")])
            kb.dma("sp", lambda q: q.dma_start(out=xt[1][:], in_=x_d[128:256, :]), wr=[R("xt4_1")])
            for i in range(NT + 1):
                b = i % 2
                if i < NT:
                    mm4(i, 0)
                    mm4(i, 1)
                if i >= 1:
                    post4a(i - 1)
                if i < NT:
                    mm4(i, 2)
                    mm4(i, 3)
                if i >= 1:
                    post4b(i - 1)
                if i == NT:
                    break
                if i + 2 < NT:
                    kb.dma("sp", lambda q: q.dma_start(out=xt[b][:], in_=x_d[(i + 2) * 128:(i + 3) * 128, :]), wr=[R(f"xt4_{b}")])
                kb.dma("sp", lambda q: q.dma_start(out=h_d[i * 128:(i + 1) * 128, :], in_=ht[b][:]), rd=[R(f"ht{b}")],
                       wr=[R(f"h_d{i}")])
                kb.op("act", lambda: nc.scalar.activation(out=junk[:], in_=ht[b][:], func=AF.Square, accum_out=ss[:, i:i + 1]),
                      rd=[R(f"ht{b}"), R("ss4")], wr=[R("junk4"), R(f"ss4_{i}")])
                kb.op("act", lambda: nc.scalar.activation(out=rstd[:, i:i + 1], in_=ss[:, i:i + 1], func=AF.Sqrt,
                                                          bias=eps_t[:, 0:1], scale=1.0 / D),
                      rd=[R(f"ss4_{i}"), R("eps_t")], wr=[R(f"rstd4_{i}")])
                kb.op("dve", lambda: nc.vector.reciprocal(out=rstd[:, i:i + 1], in_=rstd[:, i:i + 1]),
                      rd=[R(f"rstd4_{i}")], wr=[R(f"rstd4_{i}")])
                kb.op("dve", lambda: nc.vector.scalar_tensor_tensor(out=hnb[b][:], in0=ht[b][:], scalar=rstd[:, i:i + 1],
                                                                    in1=nfw[:], op0=ALU.mult, op1=ALU.mult),
                      rd=[R(f"ht{b}"), R(f"rstd4_{i}"), R("nfw")], wr=[R(f"hnb{b}")])
                kb.dma("sp", lambda q: q.dma_start(out=hn_d[i * 128:(i + 1) * 128, :], in_=hnb[b][:]), rd=[R(f"hnb{b}")],
                       wr=[R(f"hn_d{i}")])
            kb.barrier()
        p4.close()
        tap("logits", logits[:], [128, NT, 36], F32, [R("logits")])
        if stop == "p4":
            finish()
            return nc, tap_out

        wt = sbt(pB, "wt", [128, 2, NT], F32)
        sloti = sbt(pB, "sloti", [128, 2, NT], I32)
        NST = CAP // 128
        p6 = contextlib.ExitStack()
        es.enter_context(p6)
        w1s = [sbt(p6, f"w1s{i}", [128, KC, DE], BF16) for i in range(2)]
        w3s = [sbt(p6, f"w3s{i}", [128, KC, DE], BF16) for i in range(2)]
        w2s = [sbt(p6, f"w2s{i}", [128, 4, D], BF16) for i in range(2)]
        xg = [sbt(p6, f"xg{i}", [128, NST, D], BF16) for i in range(2)]
        xgT = [sbt(p6, f"xgT{i}", [128, KC, CAP], BF16) for i in range(2)]
        sil = [sbt(p6, f"sil{i}", [128, CAP], F32) for i in range(2)]
        gT = [sbt(p6, f"gT{i}", [128, 4, CAP], BF16) for i in range(2)]
        ybuf = [sbt(p6, f"ybuf{i}", [128, D], BF16) for i in range(2)]

        kb.tick = None
        conv_flush()

        def load_ew(e):
            s = e % 2
            if e in conv_set:
                for mat, wd, ws, nm in (("w1", wb1_d, w1s, "w1s"), ("w3", wb3_d, w3s, "w3s"), ("w2", wb2_d, w2s, "w2s")):
                    kb.dma("pool", lambda q: q.dma_start(out=ws[s][:].rearrange("p a n -> p (a n)"), in_=wd[e]),
                           rd=[R(f"wbu_{e}_{mat}_{q8}") for q8 in range(8)], wr=[R(f"{nm}{s}")])
                return
            kb.dma("pool", lambda q: q.dma_start(out=w1s[s][:], in_=w1_d[e].rearrange("(kc p) n -> p kc n", p=128)), wr=[R(f"w1s{s}")])
            kb.dma("pool", lambda q: q.dma_start(out=w3s[s][:], in_=w3_d[e].rearrange("(kc p) n -> p kc n", p=128)), wr=[R(f"w3s{s}")])
            kb.dma("pool", lambda q: q.dma_start(out=w2s[s][:], in_=w2_d[e].rearrange("(kc p) n -> p kc n", p=128)), wr=[R(f"w2s{s}")])

        def load_xg(e):
            s = e % 2
            kb.dma("sp", lambda q: q.dma_start(out=xg[s][:], in_=xg_d[e * CAP:(e + 1) * CAP, :].rearrange("(a p) n -> p a n", p=128)),
                   rd=[R(f"xg_d_{i}_{k}") for i in range(NT) for k in range(2)], wr=[R(f"xg{s}")])
        load_ew(0)
        load_ew(1)
        with contextlib.ExitStack() as p5:
            def t5(name, shape, dt=F32):
                return sbt(p5, name, shape, dt)
            gl = logits[:, :, 0:4]
            el = logits[:, :, 4:36]
            gmax = t5("gmax", [128, NT])
            gsh = t5("gsh", [128, NT, 4])
            gsum = t5("gsum", [128, NT])
            pgrp = t5("pgrp", [128, NT])
            ohg = t5("ohg", [128, NT, 4])
            em = t5("em", [128, NT, 32])
            m1 = t5("m1", [128, NT])
            m2 = t5("m2", [128, NT])
            A1 = t5("A1", [128, NT, 32])
            A2 = t5("A2", [128, NT, 32])
            e2 = t5("e2", [128, NT, 32])
            Ab = t5("Ab", [128, NT, 32], BF16)
            cnt = t5("cnt", [128, NT, 32])
            offs5 = t5("offs5", [128, NT, 32])
            ebase = t5("ebase", [128, NT, 32])
            tmp5 = t5("tmp5", [128, NT, 32])
            w1t = t5("w1t", [128, NT])
            cs = t5("cs", [128, 2, NT])
            eb = t5("eb", [128, 2, NT])
            okk = t5("okk", [128, 2, NT])
            slf = t5("slf", [128, 2, NT])
            kb.dma("sp", lambda q: q.dma_start(out=ebase[:].rearrange("p a b -> p (a b)"), in_=ebase_d), wr=[R("ebase")])

            def V(fn, rd, wr):
                kb.op("dve", fn, rd=[R(n) for n in rd], wr=[R(n) for n in wr])

            def bc2(ap2, n):
                return ap2.unsqueeze(2).to_broadcast([128, NT, n])
            V(lambda: nc.vector.tensor_reduce(out=gmax[:], in_=gl, axis=AX.X, op=ALU.max), ["logits"], ["gmax"])
            V(lambda: nc.vector.tensor_tensor(out=gsh[:], in0=gl, in1=bc2(gmax[:], 4), op=ALU.subtract), ["logits", "gmax"], ["gsh"])
            V(lambda: nc.vector.tensor_tensor(out=ohg[:], in0=gl, in1=bc2(gmax[:], 4), op=ALU.is_equal), ["logits", "gmax"], ["ohg"])
            kb.op("act", lambda: nc.scalar.activation(out=gsh[:], in_=gsh[:], func=AF.Exp), rd=[R("gsh")], wr=[R("gsh")])
            V(lambda: nc.vector.tensor_reduce(out=gsum[:], in_=gsh[:], axis=AX.X, op=ALU.add), ["gsh"], ["gsum"])
            V(lambda: nc.vector.reciprocal(out=pgrp[:], in_=gsum[:]), ["gsum"], ["pgrp"])
            V(lambda: nc.vector.tensor_scalar(out=ohg[:], in0=ohg[:], scalar1=1e30, scalar2=-1e30, op0=ALU.mult, op1=ALU.add),
              ["ohg"], ["ohg"])
            V(lambda: nc.vector.tensor_tensor(out=em[:].rearrange("p t (g e) -> p t g e", e=8),
                                              in0=el.rearrange("p t (g e) -> p t g e", e=8),
                                              in1=ohg[:].unsqueeze(3).to_broadcast([128, NT, 4, 8]),
                                              op=ALU.add), ["logits", "ohg"], ["em"])
            V(lambda: nc.vector.tensor_reduce(out=m1[:], in_=em[:], axis=AX.X, op=ALU.max), ["em"], ["m1"])
            V(lambda: nc.vector.tensor_tensor(out=A1[:], in0=em[:], in1=bc2(m1[:], 32), op=ALU.is_equal), ["em", "m1"], ["A1"])
            V(lambda: nc.vector.scalar_tensor_tensor(out=e2[:], in0=A1[:], scalar=-1e30, in1=em[:], op0=ALU.mult, op1=ALU.add),
              ["A1", "em"], ["e2"])
            V(lambda: nc.vector.tensor_reduce(out=m2[:], in_=e2[:], axis=AX.X, op=ALU.max), ["e2"], ["m2"])
            V(lambda: nc.vector.tensor_tensor(out=A2[:], in0=e2[:], in1=bc2(m2[:], 32), op=ALU.is_equal), ["e2", "m2"], ["A2"])
            V(lambda: nc.vector.tensor_tensor(out=w1t[:], in0=m2[:], in1=m1[:], op=ALU.subtract), ["m1", "m2"], ["w1t"])
            kb.op("act", lambda: nc.scalar.activation(out=w1t[:], in_=w1t[:], func=AF.Exp), rd=[R("w1t")], wr=[R("w1t")])
            V(lambda: nc.vector.tensor_scalar(out=w1t[:], in0=w1t[:], scalar1=1.0, scalar2=None, op0=ALU.add), ["w1t"], ["w1t"])
            V(lambda: nc.vector.reciprocal(out=w1t[:], in_=w1t[:]), ["w1t"], ["w1t"])
            V(lambda: nc.vector.tensor_tensor(out=wt[:, 0, :], in0=w1t[:], in1=pgrp[:], op=ALU.mult), ["w1t", "pgrp"], ["wt0"])
            V(lambda: nc.vector.tensor_tensor(out=wt[:, 1, :], in0=pgrp[:], in1=wt[:, 0, :], op=ALU.subtract), ["pgrp", "wt0"], ["wt1"])
            V(lambda: nc.vector.tensor_tensor(out=tmp5[:], in0=A1[:], in1=A2[:], op=ALU.add), ["A1", "A2"], ["tmp5"])
            V(lambda: nc.vector.tensor_copy(out=Ab[:], in_=tmp5[:]), ["tmp5"], ["Ab"])
            Abf = Ab[:].rearrange("p a b -> p (a b)")
            kb.op("pe", lambda: nc.tensor.matmul(ps[0][:, :], lhsT=Lstrb, rhs=Abf, start=True, stop=True), rd=[RCB, R("Ab")], wr=[Rps[0]])
            kb.op("pe", lambda: nc.tensor.matmul(ps[1][:, :], lhsT=onesb, rhs=Abf, start=True, stop=True), rd=[RCB, R("Ab")], wr=[Rps[1]])
            V(lambda: nc.vector.memset(offs5[:], 0.0), [], ["offs5"])
            for i in range(1, NT):
                kb.op("dve", lambda: nc.vector.tensor_tensor(out=offs5[:, i, :], in0=offs5[:, i - 1, :],
                                                             in1=ps[1][:, (i - 1) * 32:i * 32], op=ALU.add),
                      rd=[R("offs5"), Rps[1]], wr=[R("offs5")])
            kb.op("dve", lambda: nc.vector.tensor_tensor(out=cnt[:].rearrange("p a b -> p (a b)"), in0=ps[0][:, :],
                                                         in1=offs5[:].rearrange("p a b -> p (a b)"), op=ALU.add),
                  rd=[Rps[0], R("offs5")], wr=[R("cnt")])
            for k, Ak, nm in ((0, A1, "A1"), (1, A2, "A2")):
                V(lambda: nc.vector.tensor_tensor(out=tmp5[:], in0=Ak[:], in1=cnt[:], op=ALU.mult), [nm, "cnt"], ["tmp5"])
                V(lambda: nc.vector.tensor_reduce(out=cs[:, k, :], in_=tmp5[:], axis=AX.X, op=ALU.add), ["tmp5"], [f"cs{k}"])
                V(lambda: nc.vector.tensor_tensor(out=tmp5[:], in0=Ak[:], in1=ebase[:], op=ALU.mult), [nm, "ebase"], ["tmp5"])
                V(lambda: nc.vector.tensor_reduce(out=eb[:, k, :], in_=tmp5[:], axis=AX.X, op=ALU.add), ["tmp5"], [f"eb{k}"])
                V(lambda: nc.vector.tensor_scalar(out=okk[:, k, :], in0=cs[:, k, :], scalar1=float(CAP), scalar2=None, op0=ALU.is_lt),
                  [f"cs{k}"], [f"ok{k}"])
                V(lambda: nc.vector.tensor_tensor(out=slf[:, k, :], in0=cs[:, k, :], in1=eb[:, k, :], op=ALU.add),
                  [f"cs{k}", f"eb{k}"], [f"slf{k}"])
                V(lambda: nc.vector.tensor_scalar(out=slf[:, k, :], in0=slf[:, k, :], scalar1=-float(NSLOT), scalar2=None, op0=ALU.add),
                  [f"slf{k}"], [f"slf{k}"])
                V(lambda: nc.vector.tensor_tensor(out=slf[:, k, :], in0=slf[:, k, :], in1=okk[:, k, :], op=ALU.mult),
                  [f"slf{k}", f"ok{k}"], [f"slf{k}"])
                V(lambda: nc.vector.tensor_scalar(out=slf[:, k, :], in0=slf[:, k, :], scalar1=float(NSLOT), scalar2=None, op0=ALU.add),
                  [f"slf{k}"], [f"slf{k}"])
                V(lambda: nc.vector.tensor_copy(out=sloti[:, k, :], in_=slf[:, k, :]), [f"slf{k}"], [f"sloti{k}"])
                V(lambda: nc.vector.tensor_tensor(out=wt[:, k, :], in0=wt[:, k, :], in1=okk[:, k, :], op=ALU.mult),
                  [f"wt{k}", f"ok{k}"], [f"wt{k}"])
            tap("wt", wt[:], [128, 2, NT], F32, [R("wt0"), R("wt1")])
            tap("sloti", sloti[:], [128, 2, NT], I32, [R("sloti0"), R("sloti1")])
            zrow = t5("zrow", [128, D], BF16)
            V(lambda: nc.vector.memset(zrow[:], 0.0), [], ["zrow"])
            kb.dma("sp", lambda q: q.dma_start(out=y_d[NSLOT:NSLOT + 128, :], in_=zrow[:]), rd=[R("zrow")], wr=[R("y_dz")])
            hsc = [t5(f"hsc{i}", [128, D], BF16) for i in range(4)]
            for i in range(NT):
                b = i % 4
                kb.dma("sp", lambda q: q.dma_start(out=hsc[b][:], in_=hn_d[i * 128:(i + 1) * 128, :]), rd=[R(f"hn_d{i}")],
                       wr=[R(f"hsc{b}")])
                for k in range(2):
                    kb.dma("pool", lambda q: q.indirect_dma_start(
                        out=xg_d, out_offset=bass.IndirectOffsetOnAxis(ap=sloti[:, k, i:i + 1], axis=0),
                        in_=hsc[b][:, :], in_offset=None), rd=[R(f"hsc{b}"), R(f"sloti{k}")], wr=[R(f"xg_d_{i}_{k}")])
            if stop == "p5":
                kb.barrier()
        if stop == "p5":
            finish()
            return nc, tap_out

        if True:
            load_xg(0)
            yb_rr = 0
            for e in range(NE):
                s = e % 2
                if e + 1 < NE:
                    load_xg(e + 1)
                for a in range(NST):
                    for half in range(2):
                        bk = 6 + half
                        for k in range(8):
                            kc = half * 8 + k
                            kb.op("pe", lambda: nc.tensor.transpose(out=psb[bk][:, k * 128:(k + 1) * 128],
                                                                    in_=xg[s][:, a, kc * 128:(kc + 1) * 128], identity=identb),
                                  rd=[R(f"xg{s}"), RCB], wr=[Rps[bk]])
                        evac_copy(xgT[s][:, half * 8:(half + 1) * 8, a * 128:(a + 1) * 128],
                                  psb[bk][:, 0:1024].rearrange("p (k t) -> p k t", t=128), [Rps[bk]], [R(f"xgT{s}")], eng="dve")
                for m in range(4):
                    bk = m % 2
                    for kc in range(KC):
                        kb.op("pe", lambda: nc.tensor.matmul(ps[bk][:, 0:CAP], lhsT=w1s[s][:, kc, m * 128:(m + 1) * 128],
                                                             rhs=xgT[s][:, kc, :], start=(kc == 0), stop=(kc == KC - 1)),
                              rd=[R(f"w1s{s}"), R(f"xgT{s}")], wr=[Rps[bk]])
                    bk3 = 2 + bk
                    for kc in range(KC):
                        kb.op("pe", lambda: nc.tensor.matmul(ps[bk3][:, 0:CAP], lhsT=w3s[s][:, kc, m * 128:(m + 1) * 128],
                                                             rhs=xgT[s][:, kc, :], start=(kc == 0), stop=(kc == KC - 1)),
                              rd=[R(f"w3s{s}"), R(f"xgT{s}")], wr=[Rps[bk3]])
                    kb.op("act", lambda: nc.scalar.activation(out=sil[bk][:], in_=ps[bk][:, 0:CAP], func=AF.Silu),
                          rd=[Rps[bk]], wr=[R(f"sil{bk}")])
                    kb.op("dve", lambda: nc.vector.tensor_tensor(out=gT[s][:, m, :], in0=ps[bk3][:, 0:CAP], in1=sil[bk][:],
                                                                 op=ALU.mult), rd=[Rps[bk3], R(f"sil{bk}")], wr=[R(f"gT{s}")])
                for a in range(NST):
                    yb = yb_rr % 2
                    yb_rr += 1
                    for cg in range(4):
                        bk = 4 + (cg % 2)
                        for m in range(4):
                            kb.op("pe", lambda: nc.tensor.matmul(ps[bk][:, :], lhsT=gT[s][:, m, a * 128:(a + 1) * 128],
                                                                 rhs=w2s[s][:, m, cg * 512:(cg + 1) * 512],
                                                                 start=(m == 0), stop=(m == 3)),
                                  rd=[R(f"gT{s}"), R(f"w2s{s}")], wr=[Rps[bk]])
                        evac_copy(ybuf[yb][:, cg * 512:(cg + 1) * 512], ps[bk][:, :], [Rps[bk]], [R(f"ybuf{yb}")])
                    r0 = e * CAP + a * 128
                    kb.dma("sp", lambda q: q.dma_start(out=y_d[r0:r0 + 128, :], in_=ybuf[yb][:]), rd=[R(f"ybuf{yb}")], wr=[R(f"y_d_{e}_{a}")])
                if e + 2 < NE:
                    load_ew(e + 2)
            kb.barrier()
        p6.close()
        if stop == "p6":
            finish()
            return nc, tap_out

        with contextlib.ExitStack() as p7:
            nfin = sbt(p7, "nfin", [128, D], F32)
            kb.dma("sp", lambda q: q.dma_start(out=nfin[:], in_=nfin_d), wr=[R("nfin")])
            NB7 = 3
            hb = [sbt(p7, f"hb{i}", [128, D], F32) for i in range(NB7)]
            ob = [sbt(p7, f"ob{i}", [128, D], F32) for i in range(NB7)]
            y1 = [sbt(p7, f"y1_{i}", [128, D], BF16) for i in range(NT)]
            y2 = [sbt(p7, f"y2_{i}", [128, D], BF16) for i in range(NT)]
            for i in range(NT):
                for k, yk, nm in ((0, y1, "y1_"), (1, y2, "y2_")):
                    kb.dma("pool", lambda q: q.indirect_dma_start(
                        out=yk[i][:, :], out_offset=None, in_=y_d,
                        in_offset=bass.IndirectOffsetOnAxis(ap=sloti[:, k, i:i + 1], axis=0)),
                        rd=[R(f"y_d_{e}_{a}") for e in range(NE) for a in range(NST)] + [R("y_dz"), R(f"sloti{k}")],
                        wr=[R(f"{nm}{i}")])
            junk = sbt(p7, "junk7", [128, D], BF16)
            ss = sbt(p7, "ss7", [128, NT], F32)
            rstd = sbt(p7, "rstd7", [128, NT], F32)
            kb.op("dve", lambda: nc.vector.memset(ss[:], 0.0), wr=[R("ss7")])
            for i in range(NB7):
                kb.dma("sp", lambda q: q.dma_start(out=hb[i][:], in_=h_d[i * 128:(i + 1) * 128, :]), rd=[R(f"h_d{i}")], wr=[R(f"hb{i}")])
            for i in range(NT):
                b = i % NB7
                kb.op("dve", lambda: nc.vector.scalar_tensor_tensor(out=hb[b][:], in0=y1[i][:], scalar=wt[:, 0, i:i + 1], in1=hb[b][:],
                                                                    op0=ALU.mult, op1=ALU.add),
                      rd=[R(f"y1_{i}"), R("wt0"), R(f"hb{b}")], wr=[R(f"hb{b}")])
                kb.op("dve", lambda: nc.vector.scalar_tensor_tensor(out=hb[b][:], in0=y2[i][:], scalar=wt[:, 1, i:i + 1], in1=hb[b][:],
                                                                    op0=ALU.mult, op1=ALU.add),
                      rd=[R(f"y2_{i}"), R("wt1"), R(f"hb{b}")], wr=[R(f"hb{b}")])
                kb.op("act", lambda: nc.scalar.activation(out=junk[:], in_=hb[b][:], func=AF.Square, accum_out=ss[:, i:i + 1]),
                      rd=[R(f"hb{b}"), R("ss7")], wr=[R("junk7"), R(f"ss7_{i}")])
                kb.op("act", lambda: nc.scalar.activation(out=rstd[:, i:i + 1], in_=ss[:, i:i + 1], func=AF.Sqrt,
                                                          bias=eps_t[:, 0:1], scale=1.0 / D),
                      rd=[R(f"ss7_{i}"), R("eps_t")], wr=[R(f"rstd7_{i}")])
                kb.op("dve", lambda: nc.vector.reciprocal(out=rstd[:, i:i + 1], in_=rstd[:, i:i + 1]),
                      rd=[R(f"rstd7_{i}")], wr=[R(f"rstd7_{i}")])
                kb.op("dve", lambda: nc.vector.scalar_tensor_tensor(out=ob[b][:], in0=hb[b][:], scalar=rstd[:, i:i + 1], in1=nfin[:],
                                                                    op0=ALU.mult, op1=ALU.mult),
                      rd=[R(f"hb{b}"), R(f"rstd7_{i}"), R("nfin")], wr=[R(f"ob{b}")])
                if i + NB7 < NT:
                    j = i + NB7
                    kb.dma("sp", lambda q: q.dma_start(out=hb[b][:], in_=h_d[j * 128:(j + 1) * 128, :]), rd=[R(f"h_d{j}")], wr=[R(f"hb{b}")])
                kb.dma("sp", lambda q: q.dma_start(out=out_d[i * 128:(i + 1) * 128, :], in_=ob[b][:]), rd=[R(f"ob{b}")], wr=[Reg()])
            finish()
    return nc, tap_out


def host_consts():
    f32 = np.float32
    idx = np.arange(128)
    ident = np.eye(128, dtype=f32)
    maskT = (idx[:, None] <= idx[None, :]).astype(f32)
    lstr = (idx[:, None] < idx[None, :]).astype(f32)
    ones = np.ones((128, 128), f32)
    rswap = np.zeros((128, 128), f32)
    rswap[idx, (idx + 64) % 128] = 1.0
    sel127 = np.zeros((128, 128), f32)
    sel127[127, :] = 1.0
    cmat = np.concatenate([ident, maskT, lstr, ones, rswap, sel127], axis=1)
    hh = np.arange(NH, dtype=f32)
    log_gamma = np.log1p(-(f32(2.0) ** (-5.0 - hh))).astype(f32)
    n = np.arange(128, dtype=f32)
    diff = n[None, :] - n[:, None]
    isd = f32(1.0 / math.sqrt(HD))
    decT = np.where(diff[None] >= 0, np.exp(diff[None] * log_gamma[:, None, None]), 0.0).astype(f32) * isd
    xi = (np.exp((n[None, :] + 1.0) * log_gamma[:, None]).astype(f32) * isd)
    zeta = np.exp((128.0 - 1.0 - n[None, :]) * log_gamma[:, None]).astype(f32)
    half = 64
    inv_freq = (f32(10000.0) ** (-np.arange(half, dtype=f32) / half)).astype(f32)
    invf = np.concatenate([inv_freq, inv_freq])[:, None]
    ssign = np.concatenate([-np.ones(64, f32), np.ones(64, f32)])[:, None]
    cret = np.concatenate([
        decT.transpose(1, 0, 2).reshape(128, NH * 128),
        np.broadcast_to(xi.reshape(1, NH * 128), (128, NH * 128)),
        zeta.T, invf, ssign], axis=1).astype(f32)
    ebase = np.broadcast_to((np.arange(NE, dtype=f32) * CAP)[None, None, :], (128, NT, NE)).reshape(128, NT * NE)
    return dict(cmat=np.ascontiguousarray(cmat), cret=np.ascontiguousarray(cret), ebase=np.ascontiguousarray(ebase))


def make_in_maps(inputs, cores):
    f32 = np.float32
    c = host_consts()
    g = lambda k: np.asarray(inputs[k])
    shared = dict(
        w_in=np.ascontiguousarray(g("w_in")[0], dtype=f32),
        w_branch=np.ascontiguousarray(g("w_branch")[0], dtype=f32),
        w_out=np.ascontiguousarray(g("w_out")[0], dtype=f32),
        w_router=np.ascontiguousarray(np.concatenate([g("w_router_group")[0], g("w_router_expert")[0]], axis=1), dtype=f32),
        w1=np.ascontiguousarray(g("w1")[0], dtype=f32),
        w3=np.ascontiguousarray(g("w3")[0], dtype=f32),
        w2=np.ascontiguousarray(g("w2")[0], dtype=f32),
        nmwT=np.ascontiguousarray(g("norm_mix_w")[0].reshape(KC, 128).T, dtype=f32),
        bf_bc=np.ascontiguousarray(np.broadcast_to(np.tile(g("fox_b_f")[0], NT)[None, :], (128, 128)), dtype=f32),
        gnwT=np.ascontiguousarray(g("ret_gn_w")[0].reshape(NH, 128).T, dtype=f32),
        bgT=np.ascontiguousarray(g("b_gate")[0].reshape(32, 128).T, dtype=f32),
        nfw_bc=np.ascontiguousarray(np.broadcast_to(g("norm_ffn_w")[0][None, :], (128, D)), dtype=f32),
        nfin_bc=np.ascontiguousarray(np.broadcast_to(g("norm_final_w")[None, :], (128, D)), dtype=f32),
        rbias_bc=np.ascontiguousarray(np.broadcast_to(
            np.concatenate([g("b_router_group")[0], g("b_router_expert")[0]])[None, :], (128, 36)), dtype=f32),
        **c,
    )
    maps = []
    for b in cores:
        m = dict(shared)
        m["x"] = np.ascontiguousarray(g("x")[b], dtype=f32)
        m["pos"] = np.ascontiguousarray(g("positions")[b][None, :], dtype=np.int32)
        maps.append(m)
    return maps


_CACHE = {}


def kernel(**inputs):
    if "nc" not in _CACHE:
        _CACHE["nc"] = build()[0]
    nc = _CACHE["nc"]
    in_maps = make_in_maps(inputs, list(range(8)))
    res = run_bass_kernel_spmd(nc, in_maps, core_ids=list(range(8)))
    return np.stack([np.asarray(r["out"], dtype=np.float32) for r in res.results], axis=0)
```
